# Optimizing a Trainium2 kernel written in Bass

```python
import math
import jax, jax.numpy as jnp
from jax import lax
import numpy as np

D_MODEL = 1024
BATCH = 4
SEQ = 8192
DEPTH = 1

CONV_WIDTH = D_MODEL
CONV_K = 3
HEAD_DIM = 64
N_HEADS = 16
N_KV_HEADS = 4
GROUP = N_HEADS // N_KV_HEADS
ATTN_WIDTH = N_HEADS * HEAD_DIM
KV_WIDTH = N_KV_HEADS * HEAD_DIM
WINDOW = 128
BLOCK = 128
N_GROUPS = 4
EXPERTS_PER_GROUP = 8
N_EXPERTS = N_GROUPS * EXPERTS_PER_GROUP
TOP_K = 2
D_FF_EXPERT = 512
EPS = 1e-6

SPLIT_SIZES = [CONV_WIDTH, CONV_WIDTH, CONV_WIDTH,
               ATTN_WIDTH, KV_WIDTH, KV_WIDTH,
               D_MODEL, D_MODEL]
SPLITS = np.cumsum(SPLIT_SIZES)[:-1].tolist()
IN_COLS = int(sum(SPLIT_SIZES))

kernel_name = "hybrid_conv_swa_alibi_hmoe_block"


def rmsnorm(x, g):
    xf = x.astype(jnp.float32)
    r = lax.rsqrt(jnp.mean(xf * xf, axis=-1, keepdims=True) + EPS)
    return (xf * r).astype(x.dtype) * g


def alibi_slopes():
    return jnp.exp2(-8.0 * jnp.arange(1, N_HEADS + 1, dtype=jnp.float32) / N_HEADS)


def short_conv_mixer(b_gate, c_gate, u, conv_w, conv_b):
    cu = c_gate * u
    rhs = conv_w[:, None, :].astype(cu.dtype)
    y = lax.conv_general_dilated(cu, rhs, window_strides=(1,), padding=[(CONV_K - 1, 0)],
                                 dimension_numbers=('NWC', 'WIO', 'NWC'),
                                 feature_group_count=CONV_WIDTH)
    return b_gate * (y + conv_b)


def sliding_window_gqa(q, k, v, sinks):
    bsz, s = q.shape[0], q.shape[1]
    n = s // BLOCK
    qb = q.reshape(bsz, n, BLOCK, N_KV_HEADS, GROUP, HEAD_DIM)
    kb = k.reshape(bsz, n, BLOCK, N_KV_HEADS, HEAD_DIM)
    vb = v.reshape(bsz, n, BLOCK, N_KV_HEADS, HEAD_DIM)
    pad = ((0, 0), (1, 0), (0, 0), (0, 0), (0, 0))
    kw = jnp.concatenate([jnp.pad(kb, pad)[:, :-1], kb], axis=2)
    vw = jnp.concatenate([jnp.pad(vb, pad)[:, :-1], vb], axis=2)
    logits = jnp.einsum('bnqkgd,bnskd->bnkgqs', qb, kw).astype(jnp.float32)
    logits = logits * (1.0 / math.sqrt(HEAD_DIM))
    qi = jnp.arange(BLOCK)[:, None]
    sj = jnp.arange(2 * BLOCK)[None, :]
    dist = (qi - sj + BLOCK).astype(jnp.float32)
    key_pos = jnp.arange(n)[:, None] * BLOCK - BLOCK + sj
    valid = ((dist >= 0) & (dist < WINDOW))[None] & (key_pos >= 0)[:, None, :]
    slopes = alibi_slopes().reshape(N_KV_HEADS, GROUP)
    logits = logits - slopes[:, :, None, None] * dist
    logits = jnp.where(valid[None, :, None, None], logits, -jnp.inf)
    sink = sinks.astype(jnp.float32).reshape(N_KV_HEADS, GROUP)[None, None, :, :, None, None]
    m = jnp.maximum(jnp.max(logits, axis=-1, keepdims=True), sink)
    p = jnp.exp(logits - m)
    denom = jnp.sum(p, axis=-1, keepdims=True) + jnp.exp(sink - m)
    probs = (p / denom).astype(v.dtype)
    out = jnp.einsum('bnkgqs,bnskd->bnqkgd', probs, vw)
    return out.reshape(bsz, s, ATTN_WIDTH)


def hierarchical_moe(h, w_group, b_group, w_expert, b_expert, w_gate, w_up, w_down):
    bsz, s, d = h.shape
    t = bsz * s
    hf = h.reshape(t, d)
    gl = (hf @ w_group + b_group).astype(jnp.float32)
    gp = jax.nn.softmax(gl, axis=-1)
    g_idx = jnp.argmax(gl, axis=-1)
    p_g = jnp.take_along_axis(gp, g_idx[:, None], axis=-1)
    el = (hf @ w_expert + b_expert).astype(jnp.float32).reshape(t, N_GROUPS, EXPERTS_PER_GROUP)
    sel = jnp.take_along_axis(el, g_idx[:, None, None], axis=1)[:, 0]
    top_v, top_i = lax.top_k(sel, TOP_K)
    weights = p_g * jax.nn.softmax(top_v, axis=-1)
    expert_id = (g_idx[:, None] * EXPERTS_PER_GROUP + top_i).reshape(-1)
    order = jnp.argsort(expert_id)
    token_of = order // TOP_K
    sizes = jnp.bincount(expert_id, length=N_EXPERTS).astype(jnp.int32)
    xs = hf[token_of]
    a = lax.ragged_dot(xs, w_gate, sizes)
    b = lax.ragged_dot(xs, w_up, sizes)
    out = lax.ragged_dot(jax.nn.silu(a) * b, w_down, sizes)
    w_sorted = weights.reshape(-1)[order].astype(out.dtype)
    y = jnp.zeros((t, d), out.dtype).at[token_of].add(out * w_sorted[:, None])
    return y.reshape(bsz, s, d)


def setup_inputs(seed: int = 0) -> dict:
    key = jax.random.key(seed)
    ks = jax.random.split(key, 20)
    f32 = jnp.float32

    def nrm(k, shape, fan_in):
        return jax.random.normal(k, shape, f32) * (fan_in ** -0.5)

    def gain(k, n):
        return 1.0 + 0.02 * jax.random.normal(k, (n,), f32)

    return {
        "x": jax.random.normal(ks[0], (BATCH, SEQ, D_MODEL), f32),
        "norm_mix": gain(ks[1], D_MODEL),
        "w_in": nrm(ks[2], (D_MODEL, IN_COLS), D_MODEL),
        "conv_w": nrm(ks[3], (CONV_K, CONV_WIDTH), CONV_K),
        "conv_b": 0.02 * jax.random.normal(ks[4], (CONV_WIDTH,), f32),
        "w_a_out": nrm(ks[5], (CONV_WIDTH, D_MODEL), CONV_WIDTH),
        "sinks": 0.5 * jax.random.normal(ks[6], (N_HEADS,), f32),
        "w_b_out": nrm(ks[7], (ATTN_WIDTH, D_MODEL), ATTN_WIDTH),
        "w_o": nrm(ks[8], (D_MODEL, D_MODEL), D_MODEL),
        "norm_ffn": gain(ks[9], D_MODEL),
        "w_group": nrm(ks[10], (D_MODEL, N_GROUPS), D_MODEL),
        "b_group": 0.01 * jax.random.normal(ks[11], (N_GROUPS,), f32),
        "w_expert": nrm(ks[12], (D_MODEL, N_EXPERTS), D_MODEL),
        "b_expert": 0.01 * jax.random.normal(ks[13], (N_EXPERTS,), f32),
        "w_gate": nrm(ks[14], (N_EXPERTS, D_MODEL, D_FF_EXPERT), D_MODEL),
        "w_up": nrm(ks[15], (N_EXPERTS, D_MODEL, D_FF_EXPERT), D_MODEL),
        "w_down": nrm(ks[16], (N_EXPERTS, D_FF_EXPERT, D_MODEL), D_FF_EXPERT),
        "norm_final": gain(ks[17], D_MODEL),
    }


def reference(x, norm_mix, w_in, conv_w, conv_b, w_a_out, sinks, w_b_out, w_o,
              norm_ffn, w_group, b_group, w_expert, b_expert, w_gate, w_up, w_down,
              norm_final):
    bsz, s, _ = x.shape
    for _layer in range(DEPTH):
        h = rmsnorm(x, norm_mix)
        proj = h @ w_in
        b_gate, c_gate, u, q, k, v, g_a, g_b = jnp.split(proj, SPLITS, axis=-1)
        y_a = short_conv_mixer(b_gate, c_gate, u, conv_w, conv_b) @ w_a_out
        attn = sliding_window_gqa(q.reshape(bsz, s, N_HEADS, HEAD_DIM),
                                  k.reshape(bsz, s, N_KV_HEADS, HEAD_DIM),
                                  v.reshape(bsz, s, N_KV_HEADS, HEAD_DIM), sinks)
        y_b = attn @ w_b_out
        merged = jax.nn.sigmoid(g_a) * y_a + jax.nn.sigmoid(g_b) * y_b
        x = x + merged @ w_o
        x = x + hierarchical_moe(rmsnorm(x, norm_ffn), w_group, b_group, w_expert,
                                 b_expert, w_gate, w_up, w_down)
    return rmsnorm(x, norm_final)
```

```python
import math
from contextlib import ExitStack

import numpy as np
import concourse.bass as bass
import concourse.mybir as mybir
from concourse.bass_utils import run_bass_kernel_spmd

F32 = mybir.dt.float32
BF16 = mybir.dt.bfloat16
I32 = mybir.dt.int32
ALU = mybir.AluOpType
AF = mybir.ActivationFunctionType
AX = mybir.AxisListType

NCORES = 8
D = 1024
TOK = 4096
NTILE = TOK // 128
TB = 512
NBLK = TOK // TB
NE = 32
CAP = 384
DFF = 512
NH = 16
EPS = 1e-6
INCOLS = 6656
NEG = -1.0e9

ENGS = ("pe", "act", "dve", "pool", "sp")
P1_BLOCKS = 99


class Buf:
    def __init__(self, plan, name):
        self.name = name
        self.w = {}
        self.r = {}
        self.sem = None
        self.cnt = 0
        self.vw = 0
        self.vr = 0
        self.excl = False
        self.fdeps = {}
        plan.bufs.append(self)


class Op:
    __slots__ = ("eng", "idx", "fn", "deps", "semdeps", "dma", "inc", "val", "barrier")

    def __init__(self, eng, idx, fn):
        self.eng = eng
        self.idx = idx
        self.fn = fn
        self.deps = {}
        self.semdeps = {}
        self.dma = None
        self.inc = False
        self.val = 0
        self.barrier = False


class Plan:
    def __init__(self, nc, sems, semstack):
        self.nc = nc
        self.sems = sems
        self.semstack = semstack
        self.ops = {e: [] for e in ENGS}
        self.bufs = []
        self.pos = {e: 0 for e in ENGS}
        self.waited = {e: {p: 0 for p in ENGS} for e in ENGS}
        self.semw = {e: {} for e in ENGS}
        self.cnt = {e: 0 for e in ENGS}
        self.nbuf = 0

    def buf(self, name):
        self.nbuf += 1
        return Buf(self, "%s_%d" % (name, self.nbuf))

    def _add(self, eng, fn):
        o = Op(eng, len(self.ops[eng]), fn)
        self.ops[eng].append(o)
        return o

    def op(self, eng, fn, reads=(), writes=()):
        o = self._add(eng, fn)
        for b in reads:
            for e, i in b.w.items():
                o.deps[e] = max(o.deps.get(e, -1), i)
            if b.excl:
                for e, i in b.r.items():
                    if e != eng:
                        o.deps[e] = max(o.deps.get(e, -1), i)
            if b.vw:
                o.semdeps[b] = max(o.semdeps.get(b, 0), b.vw)
        for b in writes:
            for e, i in b.w.items():
                if e != eng or eng != "pe":
                    o.deps[e] = max(o.deps.get(e, -1), i)
            for e, i in b.r.items():
                if e != eng or eng != "pe":
                    o.deps[e] = max(o.deps.get(e, -1), i)
            if b.vr:
                o.semdeps[b] = max(o.semdeps.get(b, 0), b.vr)
            for sb_, v in b.fdeps.items():
                o.semdeps[sb_] = max(o.semdeps.get(sb_, 0), v)
        for b in reads:
            b.r[eng] = o.idx
        for b in writes:
            b.w[eng] = o.idx
        return o

    def dma(self, eng, fn, sb, sb_is_dst, reads=(), writes=(), serial=None, gate=()):
        o = self._add(eng, fn)
        o.dma = sb
        for b in gate:
            for e, i in b.w.items():
                o.deps[e] = max(o.deps.get(e, -1), i)
        if serial is not None:
            prev = getattr(serial, "last_dma", None)
            if prev is not None:
                o.semdeps[prev[0]] = max(o.semdeps.get(prev[0], 0), prev[1])
        if sb_is_dst:
            allr, allw = list(reads), list(writes) + [sb]
        else:
            allr, allw = list(reads) + [sb], list(writes)
        for b in allr:
            for e, i in b.w.items():
                o.deps[e] = max(o.deps.get(e, -1), i)
            if b.vw:
                o.semdeps[b] = max(o.semdeps.get(b, 0), b.vw)
        for b in allw:
            for e, i in b.w.items():
                o.deps[e] = max(o.deps.get(e, -1), i)
            for e, i in b.r.items():
                o.deps[e] = max(o.deps.get(e, -1), i)
            if b.vr > b.vw:
                o.semdeps[b] = max(o.semdeps.get(b, 0), b.vr)
            for sb_, v in b.fdeps.items():
                o.semdeps[sb_] = max(o.semdeps.get(sb_, 0), v)
        sb.cnt += 1
        if serial is not None:
            serial.last_dma = (sb, sb.cnt)
        for b in reads:
            if b is not sb:
                b.fdeps[sb] = sb.cnt
        if sb_is_dst:
            sb.vw = sb.cnt
        sb.vr = sb.cnt
        return o

    def barrier(self):
        lasts = {e: len(self.ops[e]) - 1 for e in ENGS}
        sems = {b: b.cnt for b in self.bufs if b.cnt}
        for e in ENGS:
            o = self._add(e, None)
            o.barrier = True
            for p, i in lasts.items():
                if p != e and i >= 0:
                    o.deps[p] = i
            o.semdeps = dict(sems)

    def _real(self, p, i):
        ops = self.ops[p]
        while i >= 0 and (ops[i].barrier or ops[i].dma is not None):
            i -= 1
        return i

    def flush(self):
        self.barrier()
        for b in self.bufs:
            if b.cnt and b.sem is None:
                b.sem = self.semstack.enter_context(self.nc.semaphore("s_" + b.name))
        for e in ENGS:
            for o in self.ops[e][self.pos[e]:]:
                for p, i in o.deps.items():
                    j = self._real(p, i)
                    if j >= self.pos[p]:
                        self.ops[p][j].inc = True
        for e in ENGS:
            c = self.cnt[e]
            for o in self.ops[e][self.pos[e]:]:
                if o.inc:
                    c += 1
                o.val = c
            self.cnt[e] = c
        P = self
        with self.nc.Block() as block:
            @block.tensor
            def _(h):
                P._run("pe", h)

            @block.scalar
            def _(h):
                P._run("act", h)

            @block.vector
            def _(h):
                P._run("dve", h)

            @block.gpsimd
            def _(h):
                P._run("pool", h)

            @block.sync
            def _(h):
                P._run("sp", h)
        for e in ENGS:
            self.pos[e] = len(self.ops[e])
        self.bufs = [b for b in self.bufs if getattr(b, "keep", False)]

    def _run(self, e, h):
        waited = self.waited[e]
        semw = self.semw[e]
        sems = self.sems
        for o in self.ops[e][self.pos[e]:]:
            for p, i in o.deps.items():
                j = self._real(p, i)
                if j < 0:
                    continue
                v = self.ops[p][j].val
                if v > waited[p]:
                    h.wait_ge(sems[p], v)
                    waited[p] = v
            for b, v in o.semdeps.items():
                if v > semw.get(b, 0):
                    h.wait_ge(b.sem, 16 * v)
                    semw[b] = v
            if o.barrier:
                continue
            ins = o.fn(h)
            if o.dma is not None:
                ins.then_inc(o.dma.sem, 16)
            elif o.inc:
                ins.then_inc(sems[e], 1)


class Rot:
    def __init__(self, items):
        self.items = items
        self.i = 0

    def next(self):
        it = self.items[self.i % len(self.items)]
        self.i += 1
        return it


def build_nc(debug=False, stop=99):
    nc = bass.Bass("TRN2", target_bir_lowering=False)

    def din(name, shape, dt=F32):
        return nc.dram_tensor(name, shape, dt, kind="ExternalInput").ap()

    xh = din("xh", [TOK + 128, D])
    w_in = din("w_in", [D, INCOLS])
    cw_d = din("cw", [128, 8, 4])
    w_a_out = din("w_a_out", [D, D])
    w_b_out = din("w_b_out", [D, D])
    w_o = din("w_o", [D, D])
    sinks_d = din("sinksrep", [128, NH])
    g1_d = din("g1rep", [128, D])
    g2_d = din("g2rep", [128, D])
    gf_d = din("gfrep", [128, D])
    wr_d = din("wr", [D, 36])
    br_d = din("brrep", [128, 36])
    w_gate = din("w_gate", [NE, D, DFF])
    w_up = din("w_up", [NE, D, DFF])
    w_down = din("w_down", [NE, DFF, D])
    biasT_d = din("biasT", [128, 8, 512])
    hmT_d = din("hmaskT", [128, 512])
    ident_d = din("identc", [128, 128])
    tri_d = din("tric", [128, 128])
    ecap_d = din("ecap", [128, NE])
    rowid_d = nc.dram_tensor("rowid", [128, NTILE * 2], I32, kind="ExternalInput").ap()
    smi_d = nc.dram_tensor("smapinit", [128, NE * CAP // 128], I32, kind="ExternalInput").ap()

    out_kind = "ExternalOutput"
    out = nc.dram_tensor("out", [TOK, D], F32, kind=out_kind).ap()
    dbg_kind = "ExternalOutput" if debug else "Internal"
    HTS = nc.dram_tensor("hts", [NBLK + 1, 128, 8 * TB], BF16, kind="Internal").ap()
    MAS = nc.dram_tensor("mas", [NBLK, 128, 8 * TB], BF16, kind="Internal").ap()
    XM = nc.dram_tensor("xm", [TOK, D], F32, kind=dbg_kind).ap()
    G = nc.dram_tensor("gbuf", [NE * CAP, D], BF16, kind="Internal").ap()
    Y2 = nc.dram_tensor("y2buf", [2 * TOK + NE * CAP, D], F32, kind="Internal").ap()
    SMAP = nc.dram_tensor("smap", [NE * CAP, 1], I32, kind="Internal").ap()
    WG16 = nc.dram_tensor("wg16", [NE, D, DFF], BF16, kind="Internal").ap()
    WD16 = nc.dram_tensor("wd16", [NE, DFF, D], BF16, kind="Internal").ap()
    WB16 = nc.dram_tensor("wb16", [128, 8, 2560], BF16, kind="Internal").ap()
    WV16 = nc.dram_tensor("wv16", [128, 8, 256], BF16, kind="Internal").ap()
    WBO16 = nc.dram_tensor("wbo16", [128, 8, D], BF16, kind="Internal").ap()
    WO16 = nc.dram_tensor("wo16", [128, 8, D], BF16, kind="Internal").ap()
    WU16 = nc.dram_tensor("wu16", [NE, D, DFF], BF16, kind="Internal").ap()
    if debug:
        POSD = nc.dram_tensor("posd", [128, NTILE * 2], I32, kind="ExternalOutput").ap()
        WTSD = nc.dram_tensor("wtsd", [128, NTILE * 2], F32, kind="ExternalOutput").ap()

    slopes = [2.0 ** (-8.0 * (h + 1) / NH) for h in range(NH)]
    w_in_v = w_in.rearrange("(k p) c -> p k c", p=128)

    with ExitStack() as top:
        sems = {e: top.enter_context(nc.semaphore("e_" + e)) for e in ENGS}
        P = Plan(nc, sems, top)

        def sbt(stack, name, shape, dt):
            t = stack.enter_context(nc.sbuf_tensor("t_" + name, shape, dt))
            return t, P.buf(name)

        ident, b_ident = sbt(top, "ident", [128, 128], BF16)
        tri, b_tri = sbt(top, "tri", [128, 128], BF16)
        ones, b_ones = sbt(top, "ones", [128, 128], BF16)
        posi, b_posi = sbt(top, "posi", [128, NTILE * 2], I32)
        wts, b_wts = sbt(top, "wts", [128, NTILE * 2], F32)
        base, b_base = sbt(top, "base", [128, NE], F32)
        rowid, b_rowid = sbt(top, "rowid", [128, NTILE * 2], I32)
        b_conv = P.buf("wconv")
        for b in (b_ident, b_tri, b_ones, b_posi, b_wts, b_base, b_rowid, b_conv):
            b.keep = True
        conv_jobs = []
        for e_ in range(NE):
            conv_jobs.append((WG16[e_].rearrange("(a b) f -> a (b f)", a=128), w_gate[e_].rearrange("(a b) f -> a (b f)", a=128)))
            conv_jobs.append((WU16[e_].rearrange("(a b) f -> a (b f)", a=128), w_up[e_].rearrange("(a b) f -> a (b f)", a=128)))
            conv_jobs.append((WD16[e_].rearrange("(a b) f -> a (b f)", a=128), w_down[e_].rearrange("(a b) f -> a (b f)", a=128)))

        p2_jobs = []
        for g in range(4):
            for hh in range(2):
                p2_jobs.append((WB16[:, :, 1024 + g * 128 + hh * 64:1024 + g * 128 + hh * 64 + 64],
                                w_in_v[:, :, 4096 + g * 64:4096 + (g + 1) * 64]))
        p2_jobs.append((WV16, w_in_v[:, :, 4352:4608]))
        for half in range(2):
            p2_jobs.append((WB16[:, :, half * 512:half * 512 + 512], w_in_v[:, :, 3072 + half * 512:3584 + half * 512]))
        for half in range(2):
            p2_jobs.append((WB16[:, :, 1536 + half * 512:2048 + half * 512],
                            w_in_v[:, :, 5632 + half * 512:6144 + half * 512]))
        p2_jobs.append((WBO16, w_b_out.rearrange("(k p) c -> p k c", p=128)))
        p2_jobs.append((WO16, w_o.rearrange("(k p) c -> p k c", p=128)))
        conv_jobs[0:0] = p2_jobs

        def emit_conv(n, gate=()):
            for _ in range(n):
                if conv_jobs:
                    o_ap, i_ap = conv_jobs.pop(0)
                    P.dma("pool", (lambda o_ap=o_ap, i_ap=i_ap: (lambda e: e.dma_start(out=o_ap, in_=i_ap)))(),
                          b_conv, True, gate=gate)

        pf = []
        for i in range(6):
            t = top.enter_context(nc.psum_tensor("pf%d" % i, [128, 512], F32))
            b = P.buf("pf%d" % i)
            b.keep = True
            b.excl = True
            pf.append((t, b))
        pbk = []
        for i in range(2):
            t = top.enter_context(nc.psum_tensor("pb%d" % i, [128, 8, 128], BF16))
            b = P.buf("pb%d" % i)
            b.keep = True
            b.excl = True
            pbk.append((t, b))
        PF = Rot(pf)
        PB = Rot(pbk)

        P.dma("pool", lambda e: e.dma_start(out=ident[:], in_=ident_d), b_ident, True)
        P.dma("pool", lambda e: e.dma_start(out=tri[:], in_=tri_d), b_tri, True)
        P.op("pool", lambda e: e.memset(ones[:], 1.0), [], [b_ones])
        P.dma("sp", lambda e: e.dma_start(out=base[:], in_=ecap_d), b_base, True)
        P.dma("pool", lambda e: e.dma_start(out=rowid[:], in_=rowid_d), b_rowid, True)

        def mm(out_ap, lhsT, rhs, start, stop, reads, writes):
            P.op("pe", lambda e: e.matmul(out=out_ap, lhsT=lhsT, rhs=rhs, start=start, stop=stop),
                 reads, writes)

        def tr(out_ap, in_ap, reads, writes):
            P.op("pe", lambda e: e.transpose(out=out_ap, in_=in_ap, identity=ident[:]),
                 list(reads) + [b_ident], writes)

        def act(out_ap, in_ap, func, reads, writes, bias=None, scale=None, accum=None):
            kw = {}
            if bias is not None:
                kw["bias"] = bias
            if scale is not None:
                kw["scale"] = scale
            if accum is not None:
                kw["accum_out"] = accum
            P.op("act", lambda e: e.activation(out=out_ap, in_=in_ap, func=func, **kw), reads, writes)

        def ts(eng, out_ap, in0, s1, s2, op0, op1, reads, writes):
            if op1 is None:
                P.op(eng, lambda e: e.tensor_scalar(out=out_ap, in0=in0, scalar1=s1, scalar2=None, op0=op0),
                     reads, writes)
            else:
                P.op(eng, lambda e: e.tensor_scalar(out=out_ap, in0=in0, scalar1=s1, scalar2=s2, op0=op0, op1=op1),
                     reads, writes)

        def tt(eng, out_ap, in0, in1, op, reads, writes):
            P.op(eng, lambda e: e.tensor_tensor(out=out_ap, in0=in0, in1=in1, op=op), reads, writes)

        def stt(eng, out_ap, in0, scalar, in1, op0, op1, reads, writes):
            P.op(eng, lambda e: e.scalar_tensor_tensor(out=out_ap, in0=in0, scalar=scalar, in1=in1, op0=op0, op1=op1),
                 reads, writes)

        def cp(eng, out_ap, in_ap, reads, writes):
            if eng == "act":
                act(out_ap, in_ap, AF.Copy, reads, writes)
            else:
                P.op(eng, lambda e: e.tensor_copy(out=out_ap, in_=in_ap), reads, writes)

        def ld(eng, out_ap, in_ap, sbuf, reads=()):
            P.dma(eng, lambda e: e.dma_start(out=out_ap, in_=in_ap), sbuf, True, reads=reads)

        def st(eng, out_ap, in_ap, sbuf):
            P.dma(eng, lambda e: e.dma_start(out=out_ap, in_=in_ap), sbuf, False)

        def rms_tile(x_ap, b_x, grep, b_g, hb_ap, b_hb, junk, b_junk, ssq, b_ss):
            act(junk[:], x_ap, AF.Square, [b_x], [b_junk, b_ss], accum=ssq[:, 0:1])
            ts("dve", ssq[:, 1:2], ssq[:, 0:1], 1.0 / D, EPS, ALU.mult, ALU.add, [b_ss], [b_ss])
            act(ssq[:, 3:4], ssq[:, 1:2], AF.Ln, [b_ss], [b_ss])
            act(ssq[:, 2:3], ssq[:, 3:4], AF.Exp, [b_ss], [b_ss], scale=-0.5)
            stt("dve", hb_ap, x_ap, ssq[:, 2:3], grep[:], ALU.mult, ALU.mult, [b_x, b_ss, b_g], [b_hb])

        with ExitStack() as s1:
            wA, b_wA = sbt(s1, "wA", [128, 8, 4096], BF16)
            waout, b_waout = sbt(s1, "waout", [128, 8, D], BF16)
            g1rep, b_g1 = sbt(s1, "g1rep", [128, D], F32)
            cw, b_cw = sbt(s1, "cw", [128, 8, 4], F32)
            zt, b_zt = sbt(s1, "zt", [128, CAP // 128, D], BF16)
            xin = Rot([sbt(s1, "xin%d" % i, [128, D], F32) for i in range(2)])
            hbr = Rot([sbt(s1, "hb%d" % i, [128, D], BF16) for i in range(4)])
            junk, b_junk = sbt(s1, "junk", [128, D], BF16)
            ssr = Rot([sbt(s1, "ss%d" % i, [128, 4], F32) for i in range(4)])
            hTr = Rot([sbt(s1, "hT%d" % i, [128, 8, TB], BF16) for i in range(2)])
            cu, b_cu = sbt(s1, "cu", [128, 8, TB + 2], F32)
            convT, b_convT = sbt(s1, "convT", [128, 8, TB], BF16)
            csb = Rot([sbt(s1, "csb%d" % i, [128, TB], F32) for i in range(2)])
            t1r = Rot([sbt(s1, "t1_%d" % i, [128, TB], F32) for i in range(2)])
            sgr = Rot([sbt(s1, "sg%d" % i, [128, TB], F32) for i in range(2)])
            mar = Rot([sbt(s1, "ma%d" % i, [128, 8, TB], BF16) for i in range(2)])

            bWA = {}
            for half in range(2):
                for (nm, c0, o0) in (("c", 1024, 1024), ("u", 2048, 2048), ("b", 0, 0)):
                    bWA[(nm, half)] = P.buf("wA_%s%d" % (nm, half))
                    P.dma("pool", (lambda c0=c0 + half * 512, o0=o0 + half * 512: (lambda e: e.dma_start(
                        out=wA[:, :, o0:o0 + 512], in_=w_in_v[:, :, c0:c0 + 512])))(), bWA[(nm, half)], True)
            stg_r = Rot([sbt(s1, "stg%d" % i, [128, 8, 256], F32) for i in range(2)])
            bWO = {}
            w_a_out_v = w_a_out.rearrange("(k p) c -> p k c", p=128)
            for j in range(4):
                for (nm, srcv, c0, dst, o0, bd) in (("ga", w_in_v, 4608, wA, 3072, bWA), ("wo", w_a_out_v, 0, waout, 0, bWO)):
                    bd[(nm, j)] = P.buf("w_%s%d" % (nm, j))
                    stg, b_stg = stg_r.next()
                    P.dma("act", (lambda stg=stg, srcv=srcv, c0=c0 + j * 256: (lambda e: e.dma_start(
                        out=stg[:], in_=srcv[:, :, c0:c0 + 256])))(), b_stg, True)
                    cp("dve" if j % 2 == 0 else "act", dst[:, :, o0 + j * 256:o0 + (j + 1) * 256], stg[:],
                       [b_stg], [bd[(nm, j)]])
            ld("sp", g1rep[:], g1_d, b_g1)
            ld("sp", cw[:], cw_d, b_cw)
            P.op("pool", lambda e: e.memset(zt[:], 0.0), [], [b_zt])
            P.op("pool", lambda e: e.memset(cu[:], 0.0), [], [b_cu])

            prepped = {}

            pst = {}

            def prep_norm(b, ntok, halo, t):
                row0 = 0 if halo else 128 + b * TB + t * 128
                x_t, b_x = xin.next()
                ld("sp", x_t[:], xh[row0:row0 + 128, :], b_x)
                hb, b_hb = hbr.next()
                ssq, b_ss = ssr.next()
                rms_tile(x_t[:], b_x, g1rep, b_g1, hb[:], b_hb, junk, b_junk, ssq, b_ss)
                pst[(b, halo, t)] = (hb, b_hb)

            def prep_tr(b, ntok, halo, t):
                if t == 0:
                    pst[(b, halo)] = hTr.next()
                hT, b_hT = pst[(b, halo)]
                hb, b_hb = pst.pop((b, halo, t))
                pb, b_pb = PB.next()
                for k in range(8):
                    tr(pb[:, k, :], hb[:, k * 128:(k + 1) * 128], [b_hb], [b_pb])
                cp("act", hT[:, :, t * 128:(t + 1) * 128], pb[:, :, :], [b_pb], [b_hT])
                if t == ntok // 128 - 1:
                    hslot = 0 if halo else b + 1
                    st("sp", HTS[hslot].rearrange("p (k t) -> p k t", k=8)[:, :, 0:ntok], hT[:, :, 0:ntok], b_hT)
                    prepped[(b, halo)] = pst.pop((b, halo))

            def pass1_block(b, ntok, halo, nxt=None):
                hT, b_hT = prepped.pop((b, halo))
                for i in range(8):
                    pc, b_pc = PF.next()
                    pu, b_pu = PF.next()
                    for k in range(8):
                        mm(pc[:, 0:ntok], wA[:, k, 1024 + i * 128:1024 + (i + 1) * 128], hT[:, k, 0:ntok],
                           k == 0, k == 7, [bWA[("c", i // 4)], b_hT], [b_pc])
                    for k in range(8):
                        mm(pu[:, 0:ntok], wA[:, k, 2048 + i * 128:2048 + (i + 1) * 128], hT[:, k, 0:ntok],
                           k == 0, k == 7, [bWA[("u", i // 4)], b_hT], [b_pu])
                    if not halo:
                        pbb, b_pbb = PF.next()
                        for k in range(8):
                            mm(pbb[:, 0:ntok], wA[:, k, i * 128:(i + 1) * 128], hT[:, k, 0:ntok],
                               k == 0, k == 7, [bWA[("b", i // 4)], b_hT], [b_pbb])
                    c_sb, b_csb = csb.next()
                    cp("act", c_sb[:, 0:ntok], pc[:, 0:ntok], [b_pc], [b_csb])
                    tt("dve", cu[:, i, 2:2 + ntok], c_sb[:, 0:ntok], pu[:, 0:ntok], ALU.mult, [b_csb, b_pu], [b_cu])
                    if not halo:
                        t1, b_t1 = t1r.next()
                        t2, b_t2 = t1r.next()
                        ts("dve", t1[:], cu[:, i, 0:ntok], cw[:, i, 0:1], cw[:, i, 3:4], ALU.mult, ALU.add,
                           [b_cu, b_cw], [b_t1])
                        stt("dve", t2[:], cu[:, i, 1:1 + ntok], cw[:, i, 1:2], t1[:], ALU.mult, ALU.add,
                            [b_cu, b_cw, b_t1], [b_t2])
                        stt("dve", t1[:], cu[:, i, 2:2 + ntok], cw[:, i, 2:3], t2[:], ALU.mult, ALU.add,
                            [b_cu, b_cw, b_t2], [b_t1])
                        tt("dve", convT[:, i, :], t1[:], pbb[:, 0:ntok], ALU.mult, [b_t1, b_pbb], [b_convT])
                    cp("dve", cu[:, i, 0:2], cu[:, i, ntok:ntok + 2], [b_cu], [b_cu])
                    if nxt is not None:
                        if i < 4:
                            prep_norm(nxt[0], nxt[1], nxt[2], i)
                        if 2 <= i < 6:
                            prep_tr(nxt[0], nxt[1], nxt[2], i - 2)
                    if i == 5 and not halo:
                        emit_conv(6, [b_hT])
                        for e_ in range(b * 4, b * 4 + 4):
                            st("sp", G[e_ * CAP:(e_ + 1) * CAP, :].rearrange("(s p) d -> p s d", p=128),
                               zt[:], b_zt)
                if halo:
                    return
                ma, b_ma = mar.next()
                for j in range(8):
                    pg, b_pg = PF.next()
                    py, b_py = PF.next()
                    for k in range(8):
                        mm(pg[:, :], wA[:, k, 3072 + j * 128:3072 + (j + 1) * 128], hT[:, k, :],
                           k == 0, k == 7, [bWA[("ga", j // 2)], b_hT], [b_pg])
                    for k in range(8):
                        mm(py[:, :], waout[:, k, j * 128:(j + 1) * 128], convT[:, k, :],
                           k == 0, k == 7, [bWO[("wo", j // 2)], b_convT], [b_py])
                    sg, b_sg = sgr.next()
                    act(sg[:], pg[:, :], AF.Tanh, [b_pg], [b_sg], scale=0.5)
                    stt("dve", ma[:, j, :], sg[:], 1.0, py[:, :], ALU.add, ALU.mult, [b_sg, b_py], [b_ma])
                st("sp", MAS[b].rearrange("p (k t) -> p k t", k=8), ma[:], b_ma)

            smi, b_smi = sbt(s1, "smi", [128, NE * CAP // 128], I32)
            ld("sp", smi[:], smi_d, b_smi)
            st("sp", SMAP.rearrange("(p f) o -> p (f o)", p=128), smi[:], b_smi)
            prep_norm(0, 128, True, 0)
            prep_tr(0, 128, True, 0)
            pass1_block(0, 128, True, nxt=(0, TB, False))
            for b in range(NBLK):
                pass1_block(b, TB, False, nxt=(b + 1, TB, False) if b + 1 < NBLK else None)
            P.flush()
        if stop <= 1:
            return nc

        with ExitStack() as s2:
            wB, b_wB = sbt(s2, "wB", [128, 8, 2560], BF16)
            wv, b_wv = sbt(s2, "wv", [128, 8, 256], BF16)
            wbout, b_wbout = sbt(s2, "wbout", [128, 8, D], BF16)
            wo, b_wo = sbt(s2, "wo", [128, 8, D], BF16)
            wr, b_wr = sbt(s2, "wr", [128, 8, 36], BF16)
            g2rep, b_g2 = sbt(s2, "g2rep", [128, D], F32)
            brep, b_br = sbt(s2, "brep", [128, 36], F32)
            sinks, b_sinks = sbt(s2, "sinks", [128, NH], F32)
            nsinks, b_nsinks = sbt(s2, "nsinks", [128, NH], F32)
            biasT, b_biasT = sbt(s2, "biasT", [128, 8, 512], BF16)
            hmT, b_hmT = sbt(s2, "hmT", [128, 512], BF16)
            hTr = Rot([sbt(s2, "hT2_%d" % i, [128, 8, TB], BF16) for i in range(2)])
            mar = Rot([sbt(s2, "ma2_%d" % i, [128, 8, TB], BF16) for i in range(1)])
            kA, b_kA = sbt(s2, "kA", [128, 4, 128 + TB], BF16)
            kB, b_kB = sbt(s2, "kB", [128, 4, 128 + TB], BF16)
            vpad = [sbt(s2, "vpad%d" % i, [128, 4, 2, 128], BF16) for i in range(5)]
            attnT, b_attnT = sbt(s2, "attnT", [128, 8, TB], BF16)
            merged, b_merged = sbt(s2, "merged", [128, 8, TB], BF16)
            qTr = Rot([sbt(s2, "qT%d" % i, [128, TB], BF16) for i in range(2)])
            pT_r = Rot([sbt(s2, "pT%d" % i, [128, 4, 128], BF16) for i in range(3)])
            den_r = Rot([sbt(s2, "den%d" % i, [128, 256], F32) for i in range(3)])
            onesAB, b_onesAB = sbt(s2, "onesAB", [128, 2, 128], BF16)
            tb_r = Rot([sbt(s2, "tb%d" % i, [128, TB], F32) for i in range(2)])
            mb_r = Rot([sbt(s2, "mb%d" % i, [128, TB], F32) for i in range(2)])
            xin = Rot([sbt(s2, "xin2_%d" % i, [128, D], F32) for i in range(2)])
            xmr = Rot([sbt(s2, "xmid%d" % i, [128, D], F32) for i in range(2)])
            h2r = Rot([sbt(s2, "h2_%d" % i, [128, D], BF16) for i in range(5)])
            h2Tr = Rot([sbt(s2, "h2T%d" % i, [128, 8, 128], BF16) for i in range(1)])
            junk, b_junk = sbt(s2, "junk2", [128, D], BF16)
            ssr = Rot([sbt(s2, "ss2_%d" % i, [128, 4], F32) for i in range(2)])
            rb_r = Rot([dict(L=sbt(s2, "rL%d" % i, [128, 4, 40], F32), W=sbt(s2, "rW%d" % i, [128, 4, 160], F32),
                             O=sbt(s2, "rO%d" % i, [128, 4, NE], BF16), Pm=sbt(s2, "rP%d" % i, [128, 4, NE], F32),
                             S=sbt(s2, "rS%d" % i, [128, 8], F32)) for i in range(2)])

            bWB = {"k": P.buf("wB_k")}
            ld("sp", wB[:, :, 1024:1536], WB16[:, :, 1024:1536], bWB["k"])
            ld("sp", wv[:], WV16, b_wv)
            for half in range(2):
                bWB[("q", half)] = P.buf("wB_q%d" % half)
                ld("sp", wB[:, :, half * 512:half * 512 + 512], WB16[:, :, half * 512:half * 512 + 512],
                   bWB[("q", half)])
            for half in range(2):
                bWB[("gb", half)] = P.buf("wB_gb%d" % half)
                ld("sp", wB[:, :, 1536 + half * 512:2048 + half * 512], WB16[:, :, 1536 + half * 512:2048 + half * 512],
                   bWB[("gb", half)])
            ld("sp", wbout[:], WBO16, b_wbout)
            ld("sp", wo[:], WO16, b_wo)
            P.dma("pool", lambda e: e.dma_start(out=wr[:], in_=wr_d.rearrange("(k p) c -> p k c", p=128)),
                  b_wr, True)
            ld("sp", g2rep[:], g2_d, b_g2)
            ld("sp", brep[:], br_d, b_br)
            ld("sp", sinks[:], sinks_d, b_sinks)
            P.dma("pool", lambda e: e.dma_start(out=biasT[:], in_=biasT_d), b_biasT, True)
            P.dma("pool", lambda e: e.dma_start(out=hmT[:], in_=hmT_d), b_hmT, True)
            for h_ in range(NH):
                c_, i_ = divmod(h_, 2)
                ts("dve", biasT[:, c_, i_ * 256:(i_ + 1) * 256], biasT[:, c_, i_ * 256:(i_ + 1) * 256],
                   sinks[:, h_:h_ + 1], None, ALU.add, None, [b_biasT, b_sinks], [b_biasT])
            P.op("pool", lambda e: e.memset(onesAB[:], 0.0), [], [b_onesAB])
            P.op("pool", lambda e: e.memset(onesAB[:, 0, 0:64], 1.0), [b_onesAB], [b_onesAB])
            P.op("pool", lambda e: e.memset(onesAB[:, 1, 64:128], 1.0), [b_onesAB], [b_onesAB])
            P.op("pool", lambda e: e.memset(kA[:], 0.0), [], [b_kA])
            P.op("pool", lambda e: e.memset(kB[:], 0.0), [], [b_kB])
            for i in range(5):
                P.op("pool", (lambda i=i: (lambda e: e.memset(vpad[i][0][:], 0.0)))(), [], [vpad[i][1]])
            for rb in rb_r.items:
                P.op("pool", (lambda t_=rb["L"][0]: (lambda e: e.memset(t_[:], -1.0e30)))(), [], [rb["L"][1]])

            def kv_part(hT, b_hT, ntok, halo, Tbase):
                koff = 0 if halo else 128
                for g in range(4):
                    pk, b_pk = PF.next()
                    for k in range(8):
                        mm(pk[:, 0:ntok], wB[:, k, 1024 + g * 128:1024 + (g + 1) * 128], hT[:, k, 0:ntok],
                           k == 0, k == 7, [bWB["k"], b_hT], [b_pk])
                    cp("act", kA[0:64, g, koff:koff + ntok], pk[0:64, 0:ntok], [b_pk], [b_kA])
                    cp("act", kB[64:128, g, koff:koff + ntok], pk[64:128, 0:ntok], [b_pk], [b_kB])
                for t in range(ntok // 128):
                    T = Tbase + t
                    vp, b_vp = vpad[(T + 1) % 5]
                    pv, b_pv = PF.next()
                    for k in range(8):
                        mm(pv[:, 0:256], hT[:, k, t * 128:(t + 1) * 128], wv[:, k, :],
                           k == 0, k == 7, [b_hT, b_wv], [b_pv])
                    pv3 = pv[:, 0:256].rearrange("p (g d) -> p g d", g=4)
                    cp("act", vp[:, :, 0, 0:64], pv3, [b_pv], [b_vp])
                    cp("dve", vp[:, :, 1, 64:128], pv3, [b_pv], [b_vp])

            def attn_stages(b, c, t, qT, b_qT):
                T = b * 4 + t
                g = c // 2
                st_ = {}

                def s1():
                    psc, b_psc = PF.next()
                    mm(psc[:, :], ident[:], biasT[:, c, :], True, False, [b_ident, b_biasT], [b_psc])
                    if T == 0:
                        mm(psc[:, :], ident[:], hmT[:], False, False, [b_ident, b_hmT], [b_psc])
                    n = 0
                    for i in range(2):
                        kk, b_kk = (kA, b_kA) if i == 0 else (kB, b_kB)
                        for kt in range(2):
                            k0 = t * 128 + kt * 128
                            mm(psc[:, (i * 2 + kt) * 128:(i * 2 + kt + 1) * 128], kk[:, g, k0:k0 + 128],
                               qT[:, t * 128:(t + 1) * 128], False, n == 3, [b_kk, b_qT], [b_psc])
                            n += 1
                    st_["psc"] = (psc, b_psc)

                def s2():
                    psc, b_psc = st_["psc"]
                    pT, b_pT = pT_r.next()
                    act(pT[:].rearrange("p a q -> p (a q)"), psc[:, :], AF.Exp, [b_psc], [b_pT], scale=-1.0)
                    st_["pT"] = (pT, b_pT)

                def s3():
                    pT, b_pT = st_["pT"]
                    po, b_po = PF.next()
                    n = 0
                    for i in range(2):
                        for kt in range(2):
                            vp, b_vp = vpad[(T + kt) % 5]
                            mm(po[:, 0:128], vp[:, g, i, :], pT[:, i * 2 + kt, :], n == 0, n == 3,
                               [b_vp, b_pT], [b_po])
                            n += 1
                    n = 0
                    for i in range(2):
                        for kt in range(2):
                            mm(po[:, 128:256], onesAB[:, i, :], pT[:, i * 2 + kt, :], n == 0, False,
                               [b_onesAB, b_pT], [b_po])
                            n += 1
                    mm(po[:, 128:256], ones[0:1, :], ones[0:1, 0:128], False, True, [b_ones], [b_po])
                    st_["po"] = (po, b_po)

                def s4():
                    po, b_po = st_["po"]
                    den, b_den = den_r.next()
                    P.op("dve", lambda e: e.reciprocal(out=den[:, 128:256], in_=po[:, 128:256]), [b_po], [b_den])
                    tt("dve", attnT[:, c, t * 128:(t + 1) * 128], po[:, 0:128], den[:, 128:256], ALU.mult,
                       [b_po, b_den], [b_attnT])

                return [s1, s2, s3, s4]

            def router_tile(T, h2, b_h2, rb):
                pb, b_pb = PB.next()
                for k in range(8):
                    tr(pb[:, k, :], h2[:, k * 128:(k + 1) * 128], [b_h2], [b_pb])
                h2T, b_h2T = h2Tr.next()
                cp("act", h2T[:], pb[:, :, :], [b_pb], [b_h2T])
                pr, b_pr = PF.next()
                for k in range(8):
                    mm(pr[:, 0:36], h2T[:, k, :], wr[:, k, :], k == 0, k == 7, [b_h2T, b_wr], [b_pr])
                L, b_L = rb["L"]
                t_ = T % 4
                tt("dve", L[:, t_, 0:4], pr[:, 0:4], brep[:, 0:4], ALU.add, [b_pr, b_br], [b_L])
                tt("dve", L[:, t_, 8:40], pr[:, 4:36], brep[:, 4:36], ALU.add, [b_pr, b_br], [b_L])

            def router_chain(b, rb, h2s):
                L, b_L = rb["L"]
                W, b_W = rb["W"]
                O, b_O = rb["O"]
                Pm, b_Pm = rb["Pm"]
                S, b_S = rb["S"]
                cst = {}
                wv_ = wts[:, 8 * b:8 * b + 8].rearrange("p (t k) -> p t k", k=2)

                def c1():
                    for t_ in range(4):
                        P.op("dve", (lambda t_=t_: (lambda e: e.max(out=W[:, t_, 0:8], in_=L[:, t_, 0:8])))(),
                             [b_L], [b_W])
                    tt("dve", W[:, :, 16:20], L[:, :, 0:4], W[:, :, 0:1].to_broadcast([128, 4, 4]), ALU.is_equal,
                       [b_L, b_W], [b_W])
                    ts("dve", W[:, :, 152:153], W[:, :, 0:1], -1.0, None, ALU.mult, None, [b_W], [b_W])
                    for t_ in range(4):
                        act(W[:, t_, 120:124], L[:, t_, 0:4], AF.Exp, [b_L, b_W], [b_W],
                            bias=W[:, t_, 152:153], accum=W[:, t_, 153:154])
                    ts("dve", W[:, :, 20:24], W[:, :, 16:20], -1.0, 1.0e30, ALU.add, ALU.mult, [b_W], [b_W])
                    for t_ in range(4):
                        tt("dve", W[:, t_, 24:56].rearrange("p (g e) -> p g e", g=4),
                           L[:, t_, 8:40].rearrange("p (g e) -> p g e", g=4),
                           W[:, t_, 20:24].unsqueeze(2).to_broadcast([128, 4, 8]), ALU.add, [b_L, b_W], [b_W])
                    for t_ in range(4):
                        P.op("dve", (lambda t_=t_: (lambda e: e.max(out=W[:, t_, 8:16], in_=W[:, t_, 24:56])))(),
                             [b_W], [b_W])
                    tt("dve", W[:, :, 56:88], W[:, :, 24:56], W[:, :, 8:9].to_broadcast([128, 4, 32]), ALU.is_equal,
                       [b_W], [b_W])
                    tt("dve", W[:, :, 88:120], W[:, :, 24:56], W[:, :, 9:10].to_broadcast([128, 4, 32]), ALU.is_equal,
                       [b_W], [b_W])
                    tt("dve", O[:], W[:, :, 56:88], W[:, :, 88:120], ALU.add, [b_W], [b_O])
                    tt("dve", W[:, :, 154:155], W[:, :, 9:10], W[:, :, 8:9], ALU.subtract, [b_W], [b_W])
                    act(W[:, :, 155:156], W[:, :, 154:155], AF.Exp, [b_W], [b_W])
                    stt("dve", W[:, :, 156:157], W[:, :, 155:156], 1.0, W[:, :, 153:154], ALU.add, ALU.mult,
                        [b_W], [b_W])
                    P.op("dve", lambda e: e.reciprocal(out=wv_[:, :, 0:1], in_=W[:, :, 156:157]), [b_W], [b_wts])
                    tt("dve", wv_[:, :, 1:2], wv_[:, :, 0:1], W[:, :, 155:156], ALU.mult, [b_wts, b_W], [b_wts])

                def c2():
                    pcm, b_pcm = PF.next()
                    for t_ in range(4):
                        mm(pcm[:, t_ * 64:t_ * 64 + 32], tri[:], O[:, t_, :], True, True, [b_tri, b_O], [b_pcm])
                        mm(pcm[:, t_ * 64 + 32:t_ * 64 + 64], ones[:], O[:, t_, :], True, True, [b_ones, b_O], [b_pcm])
                    cst["pcm"] = (pcm, b_pcm)

                def c3():
                    pcm, b_pcm = cst["pcm"]
                    for t_ in range(4):
                        tt("dve", Pm[:, t_, :], pcm[:, t_ * 64:t_ * 64 + 32], base[:], ALU.add,
                           [b_pcm, b_base], [b_Pm])
                        tt("dve", base[:], pcm[:, t_ * 64 + 32:t_ * 64 + 64], base[:], ALU.add,
                           [b_pcm, b_base], [b_base])
                    Sv = S[:, 0:8].rearrange("p (t k) -> p t k", k=2)
                    for kk, lo in ((0, 56), (1, 88)):
                        tt("dve", W[:, :, 120:152], W[:, :, lo:lo + 32], Pm[:], ALU.mult, [b_W, b_Pm], [b_W])
                        P.op("dve", (lambda kk=kk: (lambda e: e.reduce_sum(out=Sv[:, :, kk], in_=W[:, :, 120:152],
                                                                          axis=AX.X)))(), [b_W], [b_S])
                    cp("dve", posi[:, 8 * b:8 * b + 8], S[:, 0:8], [b_S], [b_posi])
                    for t_ in range(4):
                        T = b * 4 + t_
                        h2, b_h2 = h2s[t_]
                        for kk in range(2):
                            P.dma("pool", (lambda kk=kk, T=T, h2=h2: (lambda e: e.indirect_dma_start(
                                out=G, out_offset=bass.IndirectOffsetOnAxis(ap=posi[:, 2 * T + kk:2 * T + kk + 1], axis=0),
                                in_=h2[:, :], in_offset=None)))(), b_h2, False, reads=[b_posi])
                            P.dma("pool", (lambda kk=kk, T=T: (lambda e: e.indirect_dma_start(
                                out=SMAP, out_offset=bass.IndirectOffsetOnAxis(ap=posi[:, 2 * T + kk:2 * T + kk + 1], axis=0),
                                in_=rowid[:, 2 * T + kk:2 * T + kk + 1], in_offset=None)))(), b_rowid, False,
                                reads=[b_posi])

                return [c1, c2, c3]

            def pass2_block(b, halo):
                ntok = 128 if halo else TB
                hT, b_hT = hTr.next()
                hslot = 0 if halo else b + 1
                ld("sp", hT[:, :, 0:ntok], HTS[hslot].rearrange("p (k t) -> p k t", k=8)[:, :, 0:ntok], b_hT)
                if halo:
                    kv_part(hT, b_hT, ntok, True, -1)
                    return
                ma, b_ma = mar.next()
                ld("sp", ma[:], MAS[b].rearrange("p (k t) -> p k t", k=8), b_ma)
                emit_conv(8, [b_hT])
                kv_part(hT, b_hT, ntok, False, b * 4)
                def qproj(c):
                    pq, b_pq = PF.next()
                    for k in range(8):
                        mm(pq[:, :], wB[:, k, c * 128:(c + 1) * 128], hT[:, k, :], k == 0, k == 7,
                           [bWB[("q", c // 4)], b_hT], [b_pq])
                    qT, b_qT = qTr.next()
                    act(qT[:], pq[:, :], AF.Copy, [b_pq], [b_qT], scale=-0.125)
                    return qT, b_qT

                iters = []
                chains = pending_chains[:]
                del pending_chains[:]
                qcur = qproj(0)
                qnext = None
                NST = 4
                nit = 32
                for j in range(nit + NST - 1):
                    if j < nit:
                        c, t = divmod(j, 4)
                        if t == 0 and c > 0:
                            qcur = qnext
                        if t == 1 and c < 7:
                            qnext = qproj(c + 1)
                        iters.append(attn_stages(b, c, t, qcur[0], qcur[1]))
                    for k in range(NST):
                        i = j - k
                        if 0 <= i < nit:
                            iters[i][k]()
                    if chains:
                        if j == 0:
                            chains[0][0]()
                        elif j == 6:
                            chains[0][1]()
                        elif j == 8:
                            chains[0][2]()
                cp("act", kA[0:64, :, 0:128], kA[0:64, :, TB:TB + 128], [b_kA], [b_kA])
                cp("act", kB[64:128, :, 0:128], kB[64:128, :, TB:TB + 128], [b_kB], [b_kB])
                for j in range(8):
                    pg, b_pg = PF.next()
                    py, b_py = PF.next()
                    for k in range(8):
                        mm(pg[:, :], wB[:, k, 1536 + j * 128:1536 + (j + 1) * 128], hT[:, k, :],
                           k == 0, k == 7, [bWB[("gb", j // 4)], b_hT], [b_pg])
                    for k in range(8):
                        mm(py[:, :], wbout[:, k, j * 128:(j + 1) * 128], attnT[:, k, :],
                           k == 0, k == 7, [b_wbout, b_attnT], [b_py])
                    tb_, b_tb = tb_r.next()
                    act(tb_[:], pg[:, :], AF.Tanh, [b_pg], [b_tb], scale=0.5)
                    mb, b_mb = mb_r.next()
                    stt("dve", mb[:], tb_[:], 1.0, py[:, :], ALU.add, ALU.mult, [b_tb, b_py], [b_mb])
                    tt("dve", merged[:, j, :], mb[:], ma[:, j, :], ALU.add, [b_mb, b_ma], [b_merged])
                tst = [dict() for _ in range(4)]
                rb = rb_r.next()
                h2s = []

                def ta(t):
                    T = b * 4 + t
                    x_t, b_x = xin.next()
                    row0 = 128 + T * 128
                    ld("sp", x_t[:], xh[row0:row0 + 128, :], b_x)
                    xm, b_xm = xmr.next()
                    for n in range(2):
                        po, b_po = PF.next()
                        for k in range(8):
                            mm(po[:, :], merged[:, k, t * 128:(t + 1) * 128], wo[:, k, n * 512:(n + 1) * 512],
                               k == 0, k == 7, [b_merged, b_wo], [b_po])
                        stt("dve", xm[:, n * 512:(n + 1) * 512], po[:, :], 0.5, x_t[:, n * 512:(n + 1) * 512],
                            ALU.mult, ALU.add, [b_po, b_x], [b_xm])
                    st("pool", XM[T * 128:(T + 1) * 128, :], xm[:], b_xm)
                    tst[t]["xm"] = (xm, b_xm)

                def tb2(t):
                    xm, b_xm = tst[t]["xm"]
                    h2, b_h2 = h2r.next()
                    ssq, b_ss = ssr.next()
                    rms_tile(xm[:], b_xm, g2rep, b_g2, h2[:], b_h2, junk, b_junk, ssq, b_ss)
                    tst[t]["h2"] = (h2, b_h2)

                def tc(t):
                    T = b * 4 + t
                    h2, b_h2 = tst[t]["h2"]
                    router_tile(T, h2, b_h2, rb)
                    h2s.append((h2, b_h2))
                    if t == 3:
                        pending_chains.append(router_chain(b, rb, h2s))

                for step in range(6):
                    if step < 4:
                        ta(step)
                    if 0 <= step - 1 < 4:
                        tb2(step - 1)
                    if 0 <= step - 2 < 4:
                        tc(step - 2)

            pending_chains = []
            pass2_block(0, True)
            for b in range(NBLK):
                pass2_block(b, False)
            for ch in pending_chains:
                for f in ch:
                    f()
            emit_conv(len(conv_jobs))
            if debug:
                st("sp", POSD, posi[:], b_posi)
                st("sp", WTSD, wts[:], b_wts)
            P.flush()
        if stop <= 2:
            return nc

        with ExitStack() as s3:
            NS = CAP // 128
            wg_r = Rot([sbt(s3, "wg%d" % i, [128, 8, DFF], BF16) for i in range(2)])
            wu_r = Rot([sbt(s3, "wu%d" % i, [128, 8, DFF], BF16) for i in range(2)])
            wd_r = Rot([sbt(s3, "wd%d" % i, [128, 4, D], BF16) for i in range(2)])
            gt_r = Rot([sbt(s3, "gt%d" % i, [128, CAP // 128, D], BF16) for i in range(2)])
            GTr = Rot([sbt(s3, "GT%d" % i, [128, 8, CAP], BF16) for i in range(2)])
            actT, b_actT = sbt(s3, "actT", [128, 4, CAP], BF16)
            sg_r = Rot([sbt(s3, "sgB%d" % i, [128, CAP], F32) for i in range(2)])
            yb_r = Rot([sbt(s3, "yb%d" % i, [128, D], F32) for i in range(3)])
            sme_r = Rot([[sbt(s3, "sme%d_%d" % (i, j), [128, 1], I32) for j in range(NS)] for i in range(2)])

            def load_expert(e_):
                wg, b_wg = wg_r.next()
                wu, b_wu = wu_r.next()
                wd, b_wd = wd_r.next()
                gt, b_gt = gt_r.next()
                sme = sme_r.next()
                ld("sp", gt[:], G[e_ * CAP:(e_ + 1) * CAP, :].rearrange("(s p) d -> p s d", p=128), b_gt)
                ld("sp", wg[:], WG16[e_].rearrange("(k p) f -> p k f", p=128), b_wg)
                ld("sp", wu[:], WU16[e_].rearrange("(k p) f -> p k f", p=128), b_wu)
                ld("sp", wd[:], WD16[e_].rearrange("(k p) f -> p k f", p=128), b_wd)
                for j in range(NS):
                    r0 = e_ * CAP + j * 128
                    ld("sp", sme[j][0][:], SMAP[r0:r0 + 128, :], sme[j][1])
                return (wg, b_wg, wu, b_wu, wd, b_wd, gt, b_gt, sme)

            breg = {}
            casts = []
            b_Y2 = P.buf("Y2dram")

            gts = {}

            def expert_tr(e_, W, s_):
                gt, b_gt = W[6], W[7]
                if s_ == 0:
                    gts[e_] = GTr.next()
                GT, b_GT = gts[e_]
                pb, b_pb = PB.next()
                for k in range(8):
                    tr(pb[:, k, :], gt[:, s_, k * 128:(k + 1) * 128], [b_gt], [b_pb])
                cp("act" if s_ % 2 == 0 else "dve", GT[:, :, s_ * 128:(s_ + 1) * 128], pb[:, :, :],
                   [b_pb], [b_GT])

            def expert(e_, W, Wnext):
                wg, b_wg, wu, b_wu, wd, b_wd, gt, b_gt, sme = W
                GT, b_GT = gts.pop(e_)
                for f in range(4):
                    if Wnext is not None and 1 <= f <= NS:
                        expert_tr(e_ + 1, Wnext, f - 1)
                    pg, b_pg = PF.next()
                    pu, b_pu = PF.next()
                    for k in range(8):
                        mm(pg[:, 0:CAP], wg[:, k, f * 128:(f + 1) * 128], GT[:, k, :], k == 0, k == 7,
                           [b_wg, b_GT], [b_pg])
                    for k in range(8):
                        mm(pu[:, 0:CAP], wu[:, k, f * 128:(f + 1) * 128], GT[:, k, :], k == 0, k == 7,
                           [b_wu, b_GT], [b_pu])
                    sg, b_sg = sg_r.next()
                    act(sg[:], pg[:, 0:CAP], AF.Silu, [b_pg], [b_sg])
                    tt("dve", actT[:, f, :], sg[:], pu[:, 0:CAP], ALU.mult, [b_sg, b_pu], [b_actT])
                for s_ in range(NS):
                    yb, b_yb = yb_r.next()
                    for n in range(2):
                        po, b_po = PF.next()
                        for f in range(4):
                            mm(po[:, :], actT[:, f, s_ * 128:(s_ + 1) * 128], wd[:, f, n * 512:(n + 1) * 512],
                               f == 0, f == 3, [b_actT, b_wd], [b_po])
                        cp("act" if n == 0 else "dve", yb[:, n * 512:(n + 1) * 512], po[:, :], [b_po], [b_yb])
                    def scat(e, s_=s_, yb=yb):
                        return e.indirect_dma_start(
                            out=Y2, out_offset=bass.IndirectOffsetOnAxis(ap=sme[s_][0][:, :], axis=0),
                            in_=yb[:, :], in_offset=None)
                    P.dma("pool", scat, b_yb, False, reads=[sme[s_][1]])

            def do_casts():
                while casts:
                    wd, b_wd, wds, b_wds = casts.pop(0)
                    cp("act", wd[:, 0:2, :], wds[:, 0:2, :], [b_wds], [b_wd])
                    cp("dve", wd[:, 2:4, :], wds[:, 2:4, :], [b_wds], [b_wd])

            Wn = load_expert(0)
            do_casts()
            for s_ in range(NS):
                expert_tr(0, Wn, s_)
            for e_ in range(NE):
                W = Wn
                Wn = load_expert(e_ + 1) if e_ + 1 < NE else None
                expert(e_, W, Wn)
                do_casts()
            P.flush()
        if stop <= 3:
            return nc

        with ExitStack() as s4:
            gfrep, b_gf = sbt(s4, "gfrep", [128, D], F32)
            y12r = Rot([sbt(s4, "y12_%d" % i, [128, 2, D], F32) for i in range(4)])
            xmr = Rot([sbt(s4, "xmc%d" % i, [128, D], F32) for i in range(4)])
            acr = Rot([sbt(s4, "acc%d" % i, [128, D], F32) for i in range(3)])
            outr = Rot([sbt(s4, "o%d" % i, [128, D], F32) for i in range(3)])
            junk, b_junk = sbt(s4, "junk4", [128, D], BF16)
            ssr = Rot([sbt(s4, "ss4_%d" % i, [128, 4], F32) for i in range(2)])
            ld("sp", gfrep[:], gf_d, b_gf)
            loaded = {}

            def c_load(T):
                y12, b_y12 = y12r.next()
                xm, b_xm = xmr.next()
                ld("sp", y12[:], Y2[T * 256:(T + 1) * 256, :].rearrange("(p k) d -> p k d", k=2), b_y12)
                ld("sp", xm[:], XM[T * 128:(T + 1) * 128, :], b_xm)
                loaded[T] = (y12, b_y12, xm, b_xm)

            c_load(0)
            c_load(1)
            for T in range(NTILE):
                if T + 2 < NTILE:
                    c_load(T + 2)
                y12, b_y12, xm, b_xm = loaded.pop(T)
                acc, b_acc = acr.next()
                stt("dve", acc[:], y12[:, 0, :], wts[:, 2 * T:2 * T + 1], xm[:], ALU.mult, ALU.add,
                    [b_y12, b_wts, b_xm], [b_acc])
                stt("dve", acc[:], y12[:, 1, :], wts[:, 2 * T + 1:2 * T + 2], acc[:], ALU.mult, ALU.add,
                    [b_y12, b_wts, b_acc], [b_acc])
                ssq, b_ss = ssr.next()
                o, b_o = outr.next()
                act(junk[:], acc[:], AF.Square, [b_acc], [b_junk, b_ss], accum=ssq[:, 0:1])
                ts("dve", ssq[:, 1:2], ssq[:, 0:1], 1.0 / D, EPS, ALU.mult, ALU.add, [b_ss], [b_ss])
                act(ssq[:, 3:4], ssq[:, 1:2], AF.Ln, [b_ss], [b_ss])
                act(ssq[:, 2:3], ssq[:, 3:4], AF.Exp, [b_ss], [b_ss], scale=-0.5)
                stt("dve", o[:], acc[:], ssq[:, 2:3], gfrep[:], ALU.mult, ALU.mult, [b_acc, b_ss, b_gf], [b_o])
                st("sp", out[T * 128:(T + 1) * 128, :], o[:], b_o)
            P.flush()
    return nc


def _consts():
    q = np.arange(128)[:, None]
    s = np.arange(256)[None, :]
    dist = (q - s + 128).astype(np.float32)
    valid = (dist >= 0) & (dist < 128)
    slopes = 2.0 ** (-8.0 * (np.arange(NH) + 1) / NH)
    biasT = np.empty((128, 8, 512), np.float32)
    pk = np.arange(128)[:, None]
    qq = np.arange(128)[None, :]
    for c in range(8):
        for i in range(2):
            for kt in range(2):
                dkq = (qq - (kt * 128 + pk) + 128).astype(np.float32)
                ok = (dkq >= 0) & (dkq < 128)
                blk = (i * 2 + kt) * 128
                biasT[:, c, blk:blk + 128] = np.where(ok, slopes[2 * c + i] * dkq, 1.0e9)
    distm = biasT
    ident = np.eye(128, dtype=np.float32)
    tri = (np.arange(128)[:, None] < np.arange(128)[None, :]).astype(np.float32)
    ecap = np.broadcast_to((np.arange(NE) * CAP).astype(np.float32)[None, :], (128, NE)).copy()
    return distm, ident, tri, ecap


def make_in_maps(x, norm_mix, w_in, conv_w, conv_b, w_a_out, sinks, w_b_out, w_o, norm_ffn,
                 w_group, b_group, w_expert, b_expert, w_gate, w_up, w_down, norm_final):
    f = lambda a: np.ascontiguousarray(np.asarray(a, dtype=np.float32))
    x = f(x)
    bsz, seq, d = x.shape
    xf = x.reshape(bsz * seq, d)
    distm, ident, tri, ecap = _consts()
    cw = np.concatenate([f(conv_w).T, f(conv_b)[:, None]], axis=1)
    cw = np.ascontiguousarray(cw.reshape(8, 128, 4).transpose(1, 0, 2))
    rep = lambda v: np.ascontiguousarray(np.broadcast_to(f(v)[None, :], (128, f(v).shape[0])))
    wr = np.ascontiguousarray(np.concatenate([f(w_group), f(w_expert)], axis=1))
    br = rep(np.concatenate([f(b_group), f(b_expert)]))
    shared = {
        "w_in": f(w_in), "cw": cw, "w_a_out": f(w_a_out), "w_b_out": f(w_b_out), "w_o": f(w_o),
        "sinksrep": rep(sinks), "g1rep": rep(norm_mix), "g2rep": rep(norm_ffn), "gfrep": rep(norm_final),
        "wr": wr, "brrep": br, "w_gate": f(w_gate), "w_up": f(w_up), "w_down": f(w_down),
        "biasT": distm, "identc": ident, "tric": tri, "ecap": ecap,
    }
    tokidx = (np.arange(NTILE)[None, :] * 128 + np.arange(128)[:, None])
    ROWID = np.ascontiguousarray(
        (2 * tokidx[:, :, None] + np.arange(2)[None, None, :]).reshape(128, NTILE * 2).astype(np.int32))
    SMI = (2 * TOK + np.arange(NE * CAP, dtype=np.int32)).reshape(128, NE * CAP // 128)
    in_maps = []
    for c in range(NCORES):
        r0 = c * TOK
        xh = np.empty((TOK + 128, d), np.float32)
        first = (r0 % seq) == 0
        if first:
            xh[:128] = 0.0
        else:
            xh[:128] = xf[r0 - 128:r0]
        xh[128:] = xf[r0:r0 + TOK]
        m = dict(shared)
        m["rowid"] = ROWID
        m["smapinit"] = SMI
        m["xh"] = xh
        hm = np.zeros((128, 512), np.float32)
        if first:
            hm[:, 0:128] = 1.0e9
            hm[:, 256:384] = 1.0e9
        m["hmaskT"] = hm
        in_maps.append(m)
    return in_maps


_NC_CACHE = {}


def kernel(**inputs):
    x = np.asarray(inputs["x"])
    bsz, seq, d = x.shape
    assert (bsz, seq, d) == (4, 8192, 1024)
    in_maps = make_in_maps(**inputs)
    if "nc" not in _NC_CACHE:
        _NC_CACHE["nc"] = build_nc()
    nc = _NC_CACHE["nc"]
    res = run_bass_kernel_spmd(nc, in_maps, core_ids=list(range(NCORES)))
    outs = [np.asarray(r["out"], dtype=np.float32) for r in res.results]
    return np.concatenate(outs, axis=0).reshape(bsz, seq, d)
```

```python
import math
from contextlib import ExitStack

import numpy as np
import concourse.bass as bass
import concourse.mybir as mybir
from concourse.bass_utils import run_bass_kernel_spmd

F32 = mybir.dt.float32
BF16 = mybir.dt.bfloat16
I32 = mybir.dt.int32
ALU = mybir.AluOpType
AF = mybir.ActivationFunctionType
AX = mybir.AxisListType

NCORES = 8
D = 1024
TOK = 4096
NTILE = TOK // 128
TB = 512
NBLK = TOK // TB
NE = 32
CAP = 384
DFF = 512
NH = 16
EPS = 1e-6
INCOLS = 6656
NEG = -1.0e9

ENGS = ("pe", "act", "dve", "pool", "sp")
P1_BLOCKS = 99


class Buf:
    def __init__(self, plan, name):
        self.name = name
        self.w = {}
        self.r = {}
        self.sem = None
        self.cnt = 0
        self.vw = 0
        self.vr = 0
        self.excl = False
        self.fdeps = {}
        plan.bufs.append(self)


class Op:
    __slots__ = ("eng", "idx", "fn", "deps", "semdeps", "dma", "inc", "val", "barrier")

    def __init__(self, eng, idx, fn):
        self.eng = eng
        self.idx = idx
        self.fn = fn
        self.deps = {}
        self.semdeps = {}
        self.dma = None
        self.inc = False
        self.val = 0
        self.barrier = False


class Plan:
    def __init__(self, nc, sems, semstack):
        self.nc = nc
        self.sems = sems
        self.semstack = semstack
        self.ops = {e: [] for e in ENGS}
        self.bufs = []
        self.pos = {e: 0 for e in ENGS}
        self.waited = {e: {p: 0 for p in ENGS} for e in ENGS}
        self.semw = {e: {} for e in ENGS}
        self.cnt = {e: 0 for e in ENGS}
        self.nbuf = 0

    def buf(self, name):
        self.nbuf += 1
        return Buf(self, "%s_%d" % (name, self.nbuf))

    def _add(self, eng, fn):
        o = Op(eng, len(self.ops[eng]), fn)
        self.ops[eng].append(o)
        return o

    def op(self, eng, fn, reads=(), writes=()):
        o = self._add(eng, fn)
        for b in reads:
            for e, i in b.w.items():
                o.deps[e] = max(o.deps.get(e, -1), i)
            if b.excl:
                for e, i in b.r.items():
                    if e != eng:
                        o.deps[e] = max(o.deps.get(e, -1), i)
            if b.vw:
                o.semdeps[b] = max(o.semdeps.get(b, 0), b.vw)
        for b in writes:
            for e, i in b.w.items():
                if e != eng or eng != "pe":
                    o.deps[e] = max(o.deps.get(e, -1), i)
            for e, i in b.r.items():
                if e != eng or eng != "pe":
                    o.deps[e] = max(o.deps.get(e, -1), i)
            if b.vr:
                o.semdeps[b] = max(o.semdeps.get(b, 0), b.vr)
            for sb_, v in b.fdeps.items():
                o.semdeps[sb_] = max(o.semdeps.get(sb_, 0), v)
        for b in reads:
            b.r[eng] = o.idx
        for b in writes:
            b.w[eng] = o.idx
        return o

    def dma(self, eng, fn, sb, sb_is_dst, reads=(), writes=(), serial=None, gate=()):
        o = self._add(eng, fn)
        o.dma = sb
        for b in gate:
            for e, i in b.w.items():
                o.deps[e] = max(o.deps.get(e, -1), i)
        if serial is not None:
            prev = getattr(serial, "last_dma", None)
            if prev is not None:
                o.semdeps[prev[0]] = max(o.semdeps.get(prev[0], 0), prev[1])
        if sb_is_dst:
            allr, allw = list(reads), list(writes) + [sb]
        else:
            allr, allw = list(reads) + [sb], list(writes)
        for b in allr:
            for e, i in b.w.items():
                o.deps[e] = max(o.deps.get(e, -1), i)
            if b.vw:
                o.semdeps[b] = max(o.semdeps.get(b, 0), b.vw)
        for b in allw:
            for e, i in b.w.items():
                o.deps[e] = max(o.deps.get(e, -1), i)
            for e, i in b.r.items():
                o.deps[e] = max(o.deps.get(e, -1), i)
            if b.vr > b.vw:
                o.semdeps[b] = max(o.semdeps.get(b, 0), b.vr)
            for sb_, v in b.fdeps.items():
                o.semdeps[sb_] = max(o.semdeps.get(sb_, 0), v)
        sb.cnt += 1
        if serial is not None:
            serial.last_dma = (sb, sb.cnt)
        for b in reads:
            if b is not sb:
                b.fdeps[sb] = sb.cnt
        if sb_is_dst:
            sb.vw = sb.cnt
        sb.vr = sb.cnt
        return o

    def barrier(self):
        lasts = {e: len(self.ops[e]) - 1 for e in ENGS}
        sems = {b: b.cnt for b in self.bufs if b.cnt}
        for e in ENGS:
            o = self._add(e, None)
            o.barrier = True
            for p, i in lasts.items():
                if p != e and i >= 0:
                    o.deps[p] = i
            o.semdeps = dict(sems)

    def _real(self, p, i):
        ops = self.ops[p]
        while i >= 0 and (ops[i].barrier or ops[i].dma is not None):
            i -= 1
        return i

    def flush(self):
        self.barrier()
        for b in self.bufs:
            if b.cnt and b.sem is None:
                b.sem = self.semstack.enter_context(self.nc.semaphore("s_" + b.name))
        for e in ENGS:
            for o in self.ops[e][self.pos[e]:]:
                for p, i in o.deps.items():
                    j = self._real(p, i)
                    if j >= self.pos[p]:
                        self.ops[p][j].inc = True
        for e in ENGS:
            c = self.cnt[e]
            for o in self.ops[e][self.pos[e]:]:
                if o.inc:
                    c += 1
                o.val = c
            self.cnt[e] = c
        P = self
        with self.nc.Block() as block:
            @block.tensor
            def _(h):
                P._run("pe", h)

            @block.scalar
            def _(h):
                P._run("act", h)

            @block.vector
            def _(h):
                P._run("dve", h)

            @block.gpsimd
            def _(h):
                P._run("pool", h)

            @block.sync
            def _(h):
                P._run("sp", h)
        for e in ENGS:
            self.pos[e] = len(self.ops[e])
        self.bufs = [b for b in self.bufs if getattr(b, "keep", False)]

    def _run(self, e, h):
        waited = self.waited[e]
        semw = self.semw[e]
        sems = self.sems
        for o in self.ops[e][self.pos[e]:]:
            for p, i in o.deps.items():
                j = self._real(p, i)
                if j < 0:
                    continue
                v = self.ops[p][j].val
                if v > waited[p]:
                    h.wait_ge(sems[p], v)
                    waited[p] = v
            for b, v in o.semdeps.items():
                if v > semw.get(b, 0):
                    h.wait_ge(b.sem, 16 * v)
                    semw[b] = v
            if o.barrier:
                continue
            ins = o.fn(h)
            if o.dma is not None:
                ins.then_inc(o.dma.sem, 16)
            elif o.inc:
                ins.then_inc(sems[e], 1)


class Rot:
    def __init__(self, items):
        self.items = items
        self.i = 0

    def next(self):
        it = self.items[self.i % len(self.items)]
        self.i += 1
        return it


def build_nc(debug=False, stop=99):
    nc = bass.Bass("TRN2", target_bir_lowering=False)

    def din(name, shape, dt=F32):
        return nc.dram_tensor(name, shape, dt, kind="ExternalInput").ap()

    xh = din("xh", [TOK + 128, D])
    w_in = din("w_in", [D, INCOLS])
    cw_d = din("cw", [128, 8, 4])
    w_a_out = din("w_a_out", [D, D])
    w_b_out = din("w_b_out", [D, D])
    w_o = din("w_o", [D, D])
    sinks_d = din("sinksrep", [128, NH])
    g1_d = din("g1rep", [128, D])
    g2_d = din("g2rep", [128, D])
    gf_d = din("gfrep", [128, D])
    wr_d = din("wr", [D, 36])
    br_d = din("brrep", [128, 36])
    w_gate = din("w_gate", [NE, D, DFF])
    w_up = din("w_up", [NE, D, DFF])
    w_down = din("w_down", [NE, DFF, D])
    biasT_d = din("biasT", [128, 8, 512])
    hmT_d = din("hmaskT", [128, 512])
    ident_d = din("identc", [128, 128])
    tri_d = din("tric", [128, 128])
    ecap_d = din("ecap", [128, NE])
    rowid_d = nc.dram_tensor("rowid", [128, NTILE * 2], I32, kind="ExternalInput").ap()
    smi_d = nc.dram_tensor("smapinit", [128, NE * CAP // 128], I32, kind="ExternalInput").ap()

    out_kind = "ExternalOutput"
    out = nc.dram_tensor("out", [TOK, D], F32, kind=out_kind).ap()
    dbg_kind = "ExternalOutput" if debug else "Internal"
    HTS = nc.dram_tensor("hts", [NBLK + 1, 128, 8 * TB], BF16, kind="Internal").ap()
    MAS = nc.dram_tensor("mas", [NBLK, 128, 8 * TB], BF16, kind="Internal").ap()
    XM = nc.dram_tensor("xm", [TOK, D], F32, kind=dbg_kind).ap()
    G = nc.dram_tensor("gbuf", [NE * CAP, D], BF16, kind="Internal").ap()
    Y2 = nc.dram_tensor("y2buf", [2 * TOK + NE * CAP, D], F32, kind="Internal").ap()
    SMAP = nc.dram_tensor("smap", [NE * CAP, 1], I32, kind="Internal").ap()
    WG16 = nc.dram_tensor("wg16", [NE, D, DFF], BF16, kind="Internal").ap()
    WB16 = nc.dram_tensor("wb16", [128, 8, 2560], BF16, kind="Internal").ap()
    WV16 = nc.dram_tensor("wv16", [128, 8, 256], BF16, kind="Internal").ap()
    WBO16 = nc.dram_tensor("wbo16", [128, 8, D], BF16, kind="Internal").ap()
    WO16 = nc.dram_tensor("wo16", [128, 8, D], BF16, kind="Internal").ap()
    WU16 = nc.dram_tensor("wu16", [NE, D, DFF], BF16, kind="Internal").ap()
    if debug:
        POSD = nc.dram_tensor("posd", [128, NTILE * 2], I32, kind="ExternalOutput").ap()
        WTSD = nc.dram_tensor("wtsd", [128, NTILE * 2], F32, kind="ExternalOutput").ap()

    slopes = [2.0 ** (-8.0 * (h + 1) / NH) for h in range(NH)]
    w_in_v = w_in.rearrange("(k p) c -> p k c", p=128)

    with ExitStack() as top:
        sems = {e: top.enter_context(nc.semaphore("e_" + e)) for e in ENGS}
        P = Plan(nc, sems, top)

        def sbt(stack, name, shape, dt):
            t = stack.enter_context(nc.sbuf_tensor("t_" + name, shape, dt))
            return t, P.buf(name)

        ident, b_ident = sbt(top, "ident", [128, 128], BF16)
        tri, b_tri = sbt(top, "tri", [128, 128], BF16)
        ones, b_ones = sbt(top, "ones", [128, 128], BF16)
        posi, b_posi = sbt(top, "posi", [128, NTILE * 2], I32)
        wts, b_wts = sbt(top, "wts", [128, NTILE * 2], F32)
        base, b_base = sbt(top, "base", [128, NE], F32)
        rowid, b_rowid = sbt(top, "rowid", [128, NTILE * 2], I32)
        b_conv = P.buf("wconv")
        for b in (b_ident, b_tri, b_ones, b_posi, b_wts, b_base, b_rowid, b_conv):
            b.keep = True
        conv_jobs = []
        for e_ in range(NE):
            conv_jobs.append((WG16[e_], w_gate[e_]))
            conv_jobs.append((WU16[e_], w_up[e_]))

        p2_jobs = []
        for g in range(4):
            for hh in range(2):
                p2_jobs.append((WB16[:, :, 1024 + g * 128 + hh * 64:1024 + g * 128 + hh * 64 + 64],
                                w_in_v[:, :, 4096 + g * 64:4096 + (g + 1) * 64]))
        p2_jobs.append((WV16, w_in_v[:, :, 4352:4608]))
        for half in range(2):
            p2_jobs.append((WB16[:, :, half * 512:half * 512 + 512], w_in_v[:, :, 3072 + half * 512:3584 + half * 512]))
        for half in range(2):
            p2_jobs.append((WB16[:, :, 1536 + half * 512:2048 + half * 512],
                            w_in_v[:, :, 5632 + half * 512:6144 + half * 512]))
        p2_jobs.append((WBO16, w_b_out.rearrange("(k p) c -> p k c", p=128)))
        p2_jobs.append((WO16, w_o.rearrange("(k p) c -> p k c", p=128)))
        conv_jobs[0:0] = p2_jobs

        def emit_conv(n, gate=()):
            for _ in range(n):
                if conv_jobs:
                    o_ap, i_ap = conv_jobs.pop(0)
                    P.dma("pool", (lambda o_ap=o_ap, i_ap=i_ap: (lambda e: e.dma_start(out=o_ap, in_=i_ap)))(),
                          b_conv, True, gate=gate)

        pf = []
        for i in range(6):
            t = top.enter_context(nc.psum_tensor("pf%d" % i, [128, 512], F32))
            b = P.buf("pf%d" % i)
            b.keep = True
            b.excl = True
            pf.append((t, b))
        pbk = []
        for i in range(2):
            t = top.enter_context(nc.psum_tensor("pb%d" % i, [128, 8, 128], BF16))
            b = P.buf("pb%d" % i)
            b.keep = True
            b.excl = True
            pbk.append((t, b))
        PF = Rot(pf)
        PB = Rot(pbk)

        P.dma("pool", lambda e: e.dma_start(out=ident[:], in_=ident_d), b_ident, True)
        P.dma("pool", lambda e: e.dma_start(out=tri[:], in_=tri_d), b_tri, True)
        P.op("pool", lambda e: e.memset(ones[:], 1.0), [], [b_ones])
        P.dma("sp", lambda e: e.dma_start(out=base[:], in_=ecap_d), b_base, True)
        P.dma("pool", lambda e: e.dma_start(out=rowid[:], in_=rowid_d), b_rowid, True)

        def mm(out_ap, lhsT, rhs, start, stop, reads, writes):
            P.op("pe", lambda e: e.matmul(out=out_ap, lhsT=lhsT, rhs=rhs, start=start, stop=stop),
                 reads, writes)

        def tr(out_ap, in_ap, reads, writes):
            P.op("pe", lambda e: e.transpose(out=out_ap, in_=in_ap, identity=ident[:]),
                 list(reads) + [b_ident], writes)

        def act(out_ap, in_ap, func, reads, writes, bias=None, scale=None, accum=None):
            kw = {}
            if bias is not None:
                kw["bias"] = bias
            if scale is not None:
                kw["scale"] = scale
            if accum is not None:
                kw["accum_out"] = accum
            P.op("act", lambda e: e.activation(out=out_ap, in_=in_ap, func=func, **kw), reads, writes)

        def ts(eng, out_ap, in0, s1, s2, op0, op1, reads, writes):
            if op1 is None:
                P.op(eng, lambda e: e.tensor_scalar(out=out_ap, in0=in0, scalar1=s1, scalar2=None, op0=op0),
                     reads, writes)
            else:
                P.op(eng, lambda e: e.tensor_scalar(out=out_ap, in0=in0, scalar1=s1, scalar2=s2, op0=op0, op1=op1),
                     reads, writes)

        def tt(eng, out_ap, in0, in1, op, reads, writes):
            P.op(eng, lambda e: e.tensor_tensor(out=out_ap, in0=in0, in1=in1, op=op), reads, writes)

        def stt(eng, out_ap, in0, scalar, in1, op0, op1, reads, writes):
            P.op(eng, lambda e: e.scalar_tensor_tensor(out=out_ap, in0=in0, scalar=scalar, in1=in1, op0=op0, op1=op1),
                 reads, writes)

        def cp(eng, out_ap, in_ap, reads, writes):
            if eng == "act":
                act(out_ap, in_ap, AF.Copy, reads, writes)
            else:
                P.op(eng, lambda e: e.tensor_copy(out=out_ap, in_=in_ap), reads, writes)

        def ld(eng, out_ap, in_ap, sbuf, reads=()):
            P.dma(eng, lambda e: e.dma_start(out=out_ap, in_=in_ap), sbuf, True, reads=reads)

        def st(eng, out_ap, in_ap, sbuf):
            P.dma(eng, lambda e: e.dma_start(out=out_ap, in_=in_ap), sbuf, False)

        def rms_tile(x_ap, b_x, grep, b_g, hb_ap, b_hb, junk, b_junk, ssq, b_ss):
            act(junk[:], x_ap, AF.Square, [b_x], [b_junk, b_ss], accum=ssq[:, 0:1])
            ts("dve", ssq[:, 1:2], ssq[:, 0:1], 1.0 / D, EPS, ALU.mult, ALU.add, [b_ss], [b_ss])
            act(ssq[:, 3:4], ssq[:, 1:2], AF.Ln, [b_ss], [b_ss])
            act(ssq[:, 2:3], ssq[:, 3:4], AF.Exp, [b_ss], [b_ss], scale=-0.5)
            stt("dve", hb_ap, x_ap, ssq[:, 2:3], grep[:], ALU.mult, ALU.mult, [b_x, b_ss, b_g], [b_hb])

        with ExitStack() as s1:
            wA, b_wA = sbt(s1, "wA", [128, 8, 4096], BF16)
            waout, b_waout = sbt(s1, "waout", [128, 8, D], BF16)
            g1rep, b_g1 = sbt(s1, "g1rep", [128, D], F32)
            cw, b_cw = sbt(s1, "cw", [128, 8, 4], F32)
            zt, b_zt = sbt(s1, "zt", [128, CAP // 128, D], BF16)
            xin = Rot([sbt(s1, "xin%d" % i, [128, D], F32) for i in range(2)])
            hbr = Rot([sbt(s1, "hb%d" % i, [128, D], BF16) for i in range(4)])
            junk, b_junk = sbt(s1, "junk", [128, D], BF16)
            ssr = Rot([sbt(s1, "ss%d" % i, [128, 4], F32) for i in range(4)])
            hTr = Rot([sbt(s1, "hT%d" % i, [128, 8, TB], BF16) for i in range(2)])
            cu, b_cu = sbt(s1, "cu", [128, 8, TB + 2], F32)
            convT, b_convT = sbt(s1, "convT", [128, 8, TB], BF16)
            csb = Rot([sbt(s1, "csb%d" % i, [128, TB], F32) for i in range(2)])
            t1r = Rot([sbt(s1, "t1_%d" % i, [128, TB], F32) for i in range(2)])
            sgr = Rot([sbt(s1, "sg%d" % i, [128, TB], F32) for i in range(2)])
            mar = Rot([sbt(s1, "ma%d" % i, [128, 8, TB], BF16) for i in range(2)])

            bWA = {}
            for half in range(2):
                for (nm, c0, o0) in (("c", 1024, 1024), ("u", 2048, 2048), ("b", 0, 0)):
                    bWA[(nm, half)] = P.buf("wA_%s%d" % (nm, half))
                    P.dma("pool", (lambda c0=c0 + half * 512, o0=o0 + half * 512: (lambda e: e.dma_start(
                        out=wA[:, :, o0:o0 + 512], in_=w_in_v[:, :, c0:c0 + 512])))(), bWA[(nm, half)], True)
            stg_r = Rot([sbt(s1, "stg%d" % i, [128, 8, 256], F32) for i in range(2)])
            bWO = {}
            w_a_out_v = w_a_out.rearrange("(k p) c -> p k c", p=128)
            for j in range(4):
                for (nm, srcv, c0, dst, o0, bd) in (("ga", w_in_v, 4608, wA, 3072, bWA), ("wo", w_a_out_v, 0, waout, 0, bWO)):
                    bd[(nm, j)] = P.buf("w_%s%d" % (nm, j))
                    stg, b_stg = stg_r.next()
                    P.dma("act", (lambda stg=stg, srcv=srcv, c0=c0 + j * 256: (lambda e: e.dma_start(
                        out=stg[:], in_=srcv[:, :, c0:c0 + 256])))(), b_stg, True)
                    cp("dve" if j % 2 == 0 else "act", dst[:, :, o0 + j * 256:o0 + (j + 1) * 256], stg[:],
                       [b_stg], [bd[(nm, j)]])
            ld("sp", g1rep[:], g1_d, b_g1)
            ld("sp", cw[:], cw_d, b_cw)
            P.op("pool", lambda e: e.memset(zt[:], 0.0), [], [b_zt])
            P.op("pool", lambda e: e.memset(cu[:], 0.0), [], [b_cu])

            prepped = {}

            pst = {}

            def prep_norm(b, ntok, halo, t):
                row0 = 0 if halo else 128 + b * TB + t * 128
                x_t, b_x = xin.next()
                ld("sp", x_t[:], xh[row0:row0 + 128, :], b_x)
                hb, b_hb = hbr.next()
                ssq, b_ss = ssr.next()
                rms_tile(x_t[:], b_x, g1rep, b_g1, hb[:], b_hb, junk, b_junk, ssq, b_ss)
                pst[(b, halo, t)] = (hb, b_hb)

            def prep_tr(b, ntok, halo, t):
                if t == 0:
                    pst[(b, halo)] = hTr.next()
                hT, b_hT = pst[(b, halo)]
                hb, b_hb = pst.pop((b, halo, t))
                pb, b_pb = PB.next()
                for k in range(8):
                    tr(pb[:, k, :], hb[:, k * 128:(k + 1) * 128], [b_hb], [b_pb])
                cp("act", hT[:, :, t * 128:(t + 1) * 128], pb[:, :, :], [b_pb], [b_hT])
                if t == ntok // 128 - 1:
                    hslot = 0 if halo else b + 1
                    st("sp", HTS[hslot].rearrange("p (k t) -> p k t", k=8)[:, :, 0:ntok], hT[:, :, 0:ntok], b_hT)
                    prepped[(b, halo)] = pst.pop((b, halo))

            def pass1_block(b, ntok, halo, nxt=None):
                hT, b_hT = prepped.pop((b, halo))
                for i in range(8):
                    pc, b_pc = PF.next()
                    pu, b_pu = PF.next()
                    for k in range(8):
                        mm(pc[:, 0:ntok], wA[:, k, 1024 + i * 128:1024 + (i + 1) * 128], hT[:, k, 0:ntok],
                           k == 0, k == 7, [bWA[("c", i // 4)], b_hT], [b_pc])
                    for k in range(8):
                        mm(pu[:, 0:ntok], wA[:, k, 2048 + i * 128:2048 + (i + 1) * 128], hT[:, k, 0:ntok],
                           k == 0, k == 7, [bWA[("u", i // 4)], b_hT], [b_pu])
                    if not halo:
                        pbb, b_pbb = PF.next()
                        for k in range(8):
                            mm(pbb[:, 0:ntok], wA[:, k, i * 128:(i + 1) * 128], hT[:, k, 0:ntok],
                               k == 0, k == 7, [bWA[("b", i // 4)], b_hT], [b_pbb])
                    c_sb, b_csb = csb.next()
                    cp("act", c_sb[:, 0:ntok], pc[:, 0:ntok], [b_pc], [b_csb])
                    tt("dve", cu[:, i, 2:2 + ntok], c_sb[:, 0:ntok], pu[:, 0:ntok], ALU.mult, [b_csb, b_pu], [b_cu])
                    if not halo:
                        t1, b_t1 = t1r.next()
                        t2, b_t2 = t1r.next()
                        ts("dve", t1[:], cu[:, i, 0:ntok], cw[:, i, 0:1], cw[:, i, 3:4], ALU.mult, ALU.add,
                           [b_cu, b_cw], [b_t1])
                        stt("dve", t2[:], cu[:, i, 1:1 + ntok], cw[:, i, 1:2], t1[:], ALU.mult, ALU.add,
                            [b_cu, b_cw, b_t1], [b_t2])
                        stt("dve", t1[:], cu[:, i, 2:2 + ntok], cw[:, i, 2:3], t2[:], ALU.mult, ALU.add,
                            [b_cu, b_cw, b_t2], [b_t1])
                        tt("dve", convT[:, i, :], t1[:], pbb[:, 0:ntok], ALU.mult, [b_t1, b_pbb], [b_convT])
                    cp("dve", cu[:, i, 0:2], cu[:, i, ntok:ntok + 2], [b_cu], [b_cu])
                    if nxt is not None:
                        if i < 4:
                            prep_norm(nxt[0], nxt[1], nxt[2], i)
                        if 2 <= i < 6:
                            prep_tr(nxt[0], nxt[1], nxt[2], i - 2)
                    if i == 5 and not halo:
                        emit_conv(5, [b_hT])
                        for e_ in range(b * 4, b * 4 + 4):
                            st("sp", G[e_ * CAP:(e_ + 1) * CAP, :].rearrange("(s p) d -> p s d", p=128),
                               zt[:], b_zt)
                if halo:
                    return
                ma, b_ma = mar.next()
                for j in range(8):
                    pg, b_pg = PF.next()
                    py, b_py = PF.next()
                    for k in range(8):
                        mm(pg[:, :], wA[:, k, 3072 + j * 128:3072 + (j + 1) * 128], hT[:, k, :],
                           k == 0, k == 7, [bWA[("ga", j // 2)], b_hT], [b_pg])
                    for k in range(8):
                        mm(py[:, :], waout[:, k, j * 128:(j + 1) * 128], convT[:, k, :],
                           k == 0, k == 7, [bWO[("wo", j // 2)], b_convT], [b_py])
                    sg, b_sg = sgr.next()
                    act(sg[:], pg[:, :], AF.Tanh, [b_pg], [b_sg], scale=0.5)
                    stt("dve", ma[:, j, :], sg[:], 1.0, py[:, :], ALU.add, ALU.mult, [b_sg, b_py], [b_ma])
                st("sp", MAS[b].rearrange("p (k t) -> p k t", k=8), ma[:], b_ma)

            smi, b_smi = sbt(s1, "smi", [128, NE * CAP // 128], I32)
            ld("sp", smi[:], smi_d, b_smi)
            st("sp", SMAP.rearrange("(p f) o -> p (f o)", p=128), smi[:], b_smi)
            prep_norm(0, 128, True, 0)
            prep_tr(0, 128, True, 0)
            pass1_block(0, 128, True, nxt=(0, TB, False))
            for b in range(NBLK):
                pass1_block(b, TB, False, nxt=(b + 1, TB, False) if b + 1 < NBLK else None)
            P.flush()
        if stop <= 1:
            return nc

        with ExitStack() as s2:
            wB, b_wB = sbt(s2, "wB", [128, 8, 2560], BF16)
            wv, b_wv = sbt(s2, "wv", [128, 8, 256], BF16)
            wbout, b_wbout = sbt(s2, "wbout", [128, 8, D], BF16)
            wo, b_wo = sbt(s2, "wo", [128, 8, D], BF16)
            wr, b_wr = sbt(s2, "wr", [128, 8, 36], BF16)
            g2rep, b_g2 = sbt(s2, "g2rep", [128, D], F32)
            brep, b_br = sbt(s2, "brep", [128, 36], F32)
            sinks, b_sinks = sbt(s2, "sinks", [128, NH], F32)
            nsinks, b_nsinks = sbt(s2, "nsinks", [128, NH], F32)
            biasT, b_biasT = sbt(s2, "biasT", [128, 8, 512], BF16)
            hmT, b_hmT = sbt(s2, "hmT", [128, 512], BF16)
            hTr = Rot([sbt(s2, "hT2_%d" % i, [128, 8, TB], BF16) for i in range(2)])
            mar = Rot([sbt(s2, "ma2_%d" % i, [128, 8, TB], BF16) for i in range(1)])
            kA, b_kA = sbt(s2, "kA", [128, 4, 128 + TB], BF16)
            kB, b_kB = sbt(s2, "kB", [128, 4, 128 + TB], BF16)
            vpad = [sbt(s2, "vpad%d" % i, [128, 4, 2, 128], BF16) for i in range(5)]
            attnT, b_attnT = sbt(s2, "attnT", [128, 8, TB], BF16)
            merged, b_merged = sbt(s2, "merged", [128, 8, TB], BF16)
            qTr = Rot([sbt(s2, "qT%d" % i, [128, TB], BF16) for i in range(2)])
            pT_r = Rot([sbt(s2, "pT%d" % i, [128, 4, 128], BF16) for i in range(3)])
            den_r = Rot([sbt(s2, "den%d" % i, [128, 256], F32) for i in range(3)])
            onesAB, b_onesAB = sbt(s2, "onesAB", [128, 2, 128], BF16)
            tb_r = Rot([sbt(s2, "tb%d" % i, [128, TB], F32) for i in range(2)])
            mb_r = Rot([sbt(s2, "mb%d" % i, [128, TB], F32) for i in range(2)])
            xin = Rot([sbt(s2, "xin2_%d" % i, [128, D], F32) for i in range(2)])
            xmr = Rot([sbt(s2, "xmid%d" % i, [128, D], F32) for i in range(2)])
            h2r = Rot([sbt(s2, "h2_%d" % i, [128, D], BF16) for i in range(5)])
            h2Tr = Rot([sbt(s2, "h2T%d" % i, [128, 8, 128], BF16) for i in range(1)])
            junk, b_junk = sbt(s2, "junk2", [128, D], BF16)
            ssr = Rot([sbt(s2, "ss2_%d" % i, [128, 4], F32) for i in range(2)])
            rb_r = Rot([dict(L=sbt(s2, "rL%d" % i, [128, 4, 40], F32), W=sbt(s2, "rW%d" % i, [128, 4, 160], F32),
                             O=sbt(s2, "rO%d" % i, [128, 4, NE], BF16), Pm=sbt(s2, "rP%d" % i, [128, 4, NE], F32),
                             S=sbt(s2, "rS%d" % i, [128, 8], F32)) for i in range(2)])

            bWB = {"k": P.buf("wB_k")}
            ld("sp", wB[:, :, 1024:1536], WB16[:, :, 1024:1536], bWB["k"])
            ld("sp", wv[:], WV16, b_wv)
            for half in range(2):
                bWB[("q", half)] = P.buf("wB_q%d" % half)
                ld("sp", wB[:, :, half * 512:half * 512 + 512], WB16[:, :, half * 512:half * 512 + 512],
                   bWB[("q", half)])
            for half in range(2):
                bWB[("gb", half)] = P.buf("wB_gb%d" % half)
                ld("sp", wB[:, :, 1536 + half * 512:2048 + half * 512], WB16[:, :, 1536 + half * 512:2048 + half * 512],
                   bWB[("gb", half)])
            ld("sp", wbout[:], WBO16, b_wbout)
            ld("sp", wo[:], WO16, b_wo)
            P.dma("pool", lambda e: e.dma_start(out=wr[:], in_=wr_d.rearrange("(k p) c -> p k c", p=128)),
                  b_wr, True)
            ld("sp", g2rep[:], g2_d, b_g2)
            ld("sp", brep[:], br_d, b_br)
            ld("sp", sinks[:], sinks_d, b_sinks)
            P.dma("pool", lambda e: e.dma_start(out=biasT[:], in_=biasT_d), b_biasT, True)
            P.dma("pool", lambda e: e.dma_start(out=hmT[:], in_=hmT_d), b_hmT, True)
            for h_ in range(NH):
                c_, i_ = divmod(h_, 2)
                ts("dve", biasT[:, c_, i_ * 256:(i_ + 1) * 256], biasT[:, c_, i_ * 256:(i_ + 1) * 256],
                   sinks[:, h_:h_ + 1], None, ALU.add, None, [b_biasT, b_sinks], [b_biasT])
            P.op("pool", lambda e: e.memset(onesAB[:], 0.0), [], [b_onesAB])
            P.op("pool", lambda e: e.memset(onesAB[:, 0, 0:64], 1.0), [b_onesAB], [b_onesAB])
            P.op("pool", lambda e: e.memset(onesAB[:, 1, 64:128], 1.0), [b_onesAB], [b_onesAB])
            P.op("pool", lambda e: e.memset(kA[:], 0.0), [], [b_kA])
            P.op("pool", lambda e: e.memset(kB[:], 0.0), [], [b_kB])
            for i in range(5):
                P.op("pool", (lambda i=i: (lambda e: e.memset(vpad[i][0][:], 0.0)))(), [], [vpad[i][1]])
            for rb in rb_r.items:
                P.op("pool", (lambda t_=rb["L"][0]: (lambda e: e.memset(t_[:], -1.0e30)))(), [], [rb["L"][1]])

            def kv_part(hT, b_hT, ntok, halo, Tbase):
                koff = 0 if halo else 128
                for g in range(4):
                    pk, b_pk = PF.next()
                    for k in range(8):
                        mm(pk[:, 0:ntok], wB[:, k, 1024 + g * 128:1024 + (g + 1) * 128], hT[:, k, 0:ntok],
                           k == 0, k == 7, [bWB["k"], b_hT], [b_pk])
                    cp("act", kA[0:64, g, koff:koff + ntok], pk[0:64, 0:ntok], [b_pk], [b_kA])
                    cp("act", kB[64:128, g, koff:koff + ntok], pk[64:128, 0:ntok], [b_pk], [b_kB])
                for t in range(ntok // 128):
                    T = Tbase + t
                    vp, b_vp = vpad[(T + 1) % 5]
                    pv, b_pv = PF.next()
                    for k in range(8):
                        mm(pv[:, 0:256], hT[:, k, t * 128:(t + 1) * 128], wv[:, k, :],
                           k == 0, k == 7, [b_hT, b_wv], [b_pv])
                    pv3 = pv[:, 0:256].rearrange("p (g d) -> p g d", g=4)
                    cp("act", vp[:, :, 0, 0:64], pv3, [b_pv], [b_vp])
                    cp("dve", vp[:, :, 1, 64:128], pv3, [b_pv], [b_vp])

            def attn_stages(b, c, t, qT, b_qT):
                T = b * 4 + t
                g = c // 2
                st_ = {}

                def s1():
                    psc, b_psc = PF.next()
                    mm(psc[:, :], ident[:], biasT[:, c, :], True, False, [b_ident, b_biasT], [b_psc])
                    if T == 0:
                        mm(psc[:, :], ident[:], hmT[:], False, False, [b_ident, b_hmT], [b_psc])
                    n = 0
                    for i in range(2):
                        kk, b_kk = (kA, b_kA) if i == 0 else (kB, b_kB)
                        for kt in range(2):
                            k0 = t * 128 + kt * 128
                            mm(psc[:, (i * 2 + kt) * 128:(i * 2 + kt + 1) * 128], kk[:, g, k0:k0 + 128],
                               qT[:, t * 128:(t + 1) * 128], False, n == 3, [b_kk, b_qT], [b_psc])
                            n += 1
                    st_["psc"] = (psc, b_psc)

                def s2():
                    psc, b_psc = st_["psc"]
                    pT, b_pT = pT_r.next()
                    act(pT[:].rearrange("p a q -> p (a q)"), psc[:, :], AF.Exp, [b_psc], [b_pT], scale=-1.0)
                    st_["pT"] = (pT, b_pT)

                def s3():
                    pT, b_pT = st_["pT"]
                    po, b_po = PF.next()
                    n = 0
                    for i in range(2):
                        for kt in range(2):
                            vp, b_vp = vpad[(T + kt) % 5]
                            mm(po[:, 0:128], vp[:, g, i, :], pT[:, i * 2 + kt, :], n == 0, n == 3,
                               [b_vp, b_pT], [b_po])
                            n += 1
                    n = 0
                    for i in range(2):
                        for kt in range(2):
                            mm(po[:, 128:256], onesAB[:, i, :], pT[:, i * 2 + kt, :], n == 0, False,
                               [b_onesAB, b_pT], [b_po])
                            n += 1
                    mm(po[:, 128:256], ones[0:1, :], ones[0:1, 0:128], False, True, [b_ones], [b_po])
                    st_["po"] = (po, b_po)

                def s4():
                    po, b_po = st_["po"]
                    den, b_den = den_r.next()
                    P.op("dve", lambda e: e.reciprocal(out=den[:, 128:256], in_=po[:, 128:256]), [b_po], [b_den])
                    tt("dve", attnT[:, c, t * 128:(t + 1) * 128], po[:, 0:128], den[:, 128:256], ALU.mult,
                       [b_po, b_den], [b_attnT])

                return [s1, s2, s3, s4]

            def router_tile(T, h2, b_h2, rb):
                pb, b_pb = PB.next()
                for k in range(8):
                    tr(pb[:, k, :], h2[:, k * 128:(k + 1) * 128], [b_h2], [b_pb])
                h2T, b_h2T = h2Tr.next()
                cp("act", h2T[:], pb[:, :, :], [b_pb], [b_h2T])
                pr, b_pr = PF.next()
                for k in range(8):
                    mm(pr[:, 0:36], h2T[:, k, :], wr[:, k, :], k == 0, k == 7, [b_h2T, b_wr], [b_pr])
                L, b_L = rb["L"]
                t_ = T % 4
                tt("dve", L[:, t_, 0:4], pr[:, 0:4], brep[:, 0:4], ALU.add, [b_pr, b_br], [b_L])
                tt("dve", L[:, t_, 8:40], pr[:, 4:36], brep[:, 4:36], ALU.add, [b_pr, b_br], [b_L])

            def router_chain(b, rb, h2s):
                L, b_L = rb["L"]
                W, b_W = rb["W"]
                O, b_O = rb["O"]
                Pm, b_Pm = rb["Pm"]
                S, b_S = rb["S"]
                cst = {}
                wv_ = wts[:, 8 * b:8 * b + 8].rearrange("p (t k) -> p t k", k=2)

                def c1():
                    for t_ in range(4):
                        P.op("dve", (lambda t_=t_: (lambda e: e.max(out=W[:, t_, 0:8], in_=L[:, t_, 0:8])))(),
                             [b_L], [b_W])
                    tt("dve", W[:, :, 16:20], L[:, :, 0:4], W[:, :, 0:1].to_broadcast([128, 4, 4]), ALU.is_equal,
                       [b_L, b_W], [b_W])
                    ts("dve", W[:, :, 152:153], W[:, :, 0:1], -1.0, None, ALU.mult, None, [b_W], [b_W])
                    for t_ in range(4):
                        act(W[:, t_, 120:124], L[:, t_, 0:4], AF.Exp, [b_L, b_W], [b_W],
                            bias=W[:, t_, 152:153], accum=W[:, t_, 153:154])
                    ts("dve", W[:, :, 20:24], W[:, :, 16:20], -1.0, 1.0e30, ALU.add, ALU.mult, [b_W], [b_W])
                    for t_ in range(4):
                        tt("dve", W[:, t_, 24:56].rearrange("p (g e) -> p g e", g=4),
                           L[:, t_, 8:40].rearrange("p (g e) -> p g e", g=4),
                           W[:, t_, 20:24].unsqueeze(2).to_broadcast([128, 4, 8]), ALU.add, [b_L, b_W], [b_W])
                    for t_ in range(4):
                        P.op("dve", (lambda t_=t_: (lambda e: e.max(out=W[:, t_, 8:16], in_=W[:, t_, 24:56])))(),
                             [b_W], [b_W])
                    tt("dve", W[:, :, 56:88], W[:, :, 24:56], W[:, :, 8:9].to_broadcast([128, 4, 32]), ALU.is_equal,
                       [b_W], [b_W])
                    tt("dve", W[:, :, 88:120], W[:, :, 24:56], W[:, :, 9:10].to_broadcast([128, 4, 32]), ALU.is_equal,
                       [b_W], [b_W])
                    tt("dve", O[:], W[:, :, 56:88], W[:, :, 88:120], ALU.add, [b_W], [b_O])
                    tt("dve", W[:, :, 154:155], W[:, :, 9:10], W[:, :, 8:9], ALU.subtract, [b_W], [b_W])
                    act(W[:, :, 155:156], W[:, :, 154:155], AF.Exp, [b_W], [b_W])
                    stt("dve", W[:, :, 156:157], W[:, :, 155:156], 1.0, W[:, :, 153:154], ALU.add, ALU.mult,
                        [b_W], [b_W])
                    P.op("dve", lambda e: e.reciprocal(out=wv_[:, :, 0:1], in_=W[:, :, 156:157]), [b_W], [b_wts])
                    tt("dve", wv_[:, :, 1:2], wv_[:, :, 0:1], W[:, :, 155:156], ALU.mult, [b_wts, b_W], [b_wts])

                def c2():
                    pcm, b_pcm = PF.next()
                    for t_ in range(4):
                        mm(pcm[:, t_ * 64:t_ * 64 + 32], tri[:], O[:, t_, :], True, True, [b_tri, b_O], [b_pcm])
                        mm(pcm[:, t_ * 64 + 32:t_ * 64 + 64], ones[:], O[:, t_, :], True, True, [b_ones, b_O], [b_pcm])
                    cst["pcm"] = (pcm, b_pcm)

                def c3():
                    pcm, b_pcm = cst["pcm"]
                    for t_ in range(4):
                        tt("dve", Pm[:, t_, :], pcm[:, t_ * 64:t_ * 64 + 32], base[:], ALU.add,
                           [b_pcm, b_base], [b_Pm])
                        tt("dve", base[:], pcm[:, t_ * 64 + 32:t_ * 64 + 64], base[:], ALU.add,
                           [b_pcm, b_base], [b_base])
                    Sv = S[:, 0:8].rearrange("p (t k) -> p t k", k=2)
                    for kk, lo in ((0, 56), (1, 88)):
                        tt("dve", W[:, :, 120:152], W[:, :, lo:lo + 32], Pm[:], ALU.mult, [b_W, b_Pm], [b_W])
                        P.op("dve", (lambda kk=kk: (lambda e: e.reduce_sum(out=Sv[:, :, kk], in_=W[:, :, 120:152],
                                                                          axis=AX.X)))(), [b_W], [b_S])
                    cp("dve", posi[:, 8 * b:8 * b + 8], S[:, 0:8], [b_S], [b_posi])
                    for t_ in range(4):
                        T = b * 4 + t_
                        h2, b_h2 = h2s[t_]
                        for kk in range(2):
                            P.dma("pool", (lambda kk=kk, T=T, h2=h2: (lambda e: e.indirect_dma_start(
                                out=G, out_offset=bass.IndirectOffsetOnAxis(ap=posi[:, 2 * T + kk:2 * T + kk + 1], axis=0),
                                in_=h2[:, :], in_offset=None)))(), b_h2, False, reads=[b_posi])
                            P.dma("pool", (lambda kk=kk, T=T: (lambda e: e.indirect_dma_start(
                                out=SMAP, out_offset=bass.IndirectOffsetOnAxis(ap=posi[:, 2 * T + kk:2 * T + kk + 1], axis=0),
                                in_=rowid[:, 2 * T + kk:2 * T + kk + 1], in_offset=None)))(), b_rowid, False,
                                reads=[b_posi])

                return [c1, c2, c3]

            def pass2_block(b, halo):
                ntok = 128 if halo else TB
                hT, b_hT = hTr.next()
                hslot = 0 if halo else b + 1
                ld("sp", hT[:, :, 0:ntok], HTS[hslot].rearrange("p (k t) -> p k t", k=8)[:, :, 0:ntok], b_hT)
                if halo:
                    kv_part(hT, b_hT, ntok, True, -1)
                    return
                ma, b_ma = mar.next()
                ld("sp", ma[:], MAS[b].rearrange("p (k t) -> p k t", k=8), b_ma)
                emit_conv(5, [b_hT])
                kv_part(hT, b_hT, ntok, False, b * 4)
                def qproj(c):
                    pq, b_pq = PF.next()
                    for k in range(8):
                        mm(pq[:, :], wB[:, k, c * 128:(c + 1) * 128], hT[:, k, :], k == 0, k == 7,
                           [bWB[("q", c // 4)], b_hT], [b_pq])
                    qT, b_qT = qTr.next()
                    act(qT[:], pq[:, :], AF.Copy, [b_pq], [b_qT], scale=-0.125)
                    return qT, b_qT

                iters = []
                chains = pending_chains[:]
                del pending_chains[:]
                qcur = qproj(0)
                qnext = None
                NST = 4
                nit = 32
                for j in range(nit + NST - 1):
                    if j < nit:
                        c, t = divmod(j, 4)
                        if t == 0 and c > 0:
                            qcur = qnext
                        if t == 1 and c < 7:
                            qnext = qproj(c + 1)
                        iters.append(attn_stages(b, c, t, qcur[0], qcur[1]))
                    for k in range(NST):
                        i = j - k
                        if 0 <= i < nit:
                            iters[i][k]()
                    if chains:
                        if j == 0:
                            chains[0][0]()
                        elif j == 6:
                            chains[0][1]()
                        elif j == 8:
                            chains[0][2]()
                cp("act", kA[0:64, :, 0:128], kA[0:64, :, TB:TB + 128], [b_kA], [b_kA])
                cp("act", kB[64:128, :, 0:128], kB[64:128, :, TB:TB + 128], [b_kB], [b_kB])
                for j in range(8):
                    pg, b_pg = PF.next()
                    py, b_py = PF.next()
                    for k in range(8):
                        mm(pg[:, :], wB[:, k, 1536 + j * 128:1536 + (j + 1) * 128], hT[:, k, :],
                           k == 0, k == 7, [bWB[("gb", j // 4)], b_hT], [b_pg])
                    for k in range(8):
                        mm(py[:, :], wbout[:, k, j * 128:(j + 1) * 128], attnT[:, k, :],
                           k == 0, k == 7, [b_wbout, b_attnT], [b_py])
                    tb_, b_tb = tb_r.next()
                    act(tb_[:], pg[:, :], AF.Tanh, [b_pg], [b_tb], scale=0.5)
                    mb, b_mb = mb_r.next()
                    stt("dve", mb[:], tb_[:], 1.0, py[:, :], ALU.add, ALU.mult, [b_tb, b_py], [b_mb])
                    tt("dve", merged[:, j, :], mb[:], ma[:, j, :], ALU.add, [b_mb, b_ma], [b_merged])
                tst = [dict() for _ in range(4)]
                rb = rb_r.next()
                h2s = []

                def ta(t):
                    T = b * 4 + t
                    x_t, b_x = xin.next()
                    row0 = 128 + T * 128
                    ld("sp", x_t[:], xh[row0:row0 + 128, :], b_x)
                    xm, b_xm = xmr.next()
                    for n in range(2):
                        po, b_po = PF.next()
                        for k in range(8):
                            mm(po[:, :], merged[:, k, t * 128:(t + 1) * 128], wo[:, k, n * 512:(n + 1) * 512],
                               k == 0, k == 7, [b_merged, b_wo], [b_po])
                        stt("dve", xm[:, n * 512:(n + 1) * 512], po[:, :], 0.5, x_t[:, n * 512:(n + 1) * 512],
                            ALU.mult, ALU.add, [b_po, b_x], [b_xm])
                    st("pool", XM[T * 128:(T + 1) * 128, :], xm[:], b_xm)
                    tst[t]["xm"] = (xm, b_xm)

                def tb2(t):
                    xm, b_xm = tst[t]["xm"]
                    h2, b_h2 = h2r.next()
                    ssq, b_ss = ssr.next()
                    rms_tile(xm[:], b_xm, g2rep, b_g2, h2[:], b_h2, junk, b_junk, ssq, b_ss)
                    tst[t]["h2"] = (h2, b_h2)

                def tc(t):
                    T = b * 4 + t
                    h2, b_h2 = tst[t]["h2"]
                    router_tile(T, h2, b_h2, rb)
                    h2s.append((h2, b_h2))
                    if t == 3:
                        pending_chains.append(router_chain(b, rb, h2s))

                for step in range(6):
                    if step < 4:
                        ta(step)
                    if 0 <= step - 1 < 4:
                        tb2(step - 1)
                    if 0 <= step - 2 < 4:
                        tc(step - 2)

            pending_chains = []
            pass2_block(0, True)
            for b in range(NBLK):
                pass2_block(b, False)
            for ch in pending_chains:
                for f in ch:
                    f()
            emit_conv(len(conv_jobs))
            if debug:
                st("sp", POSD, posi[:], b_posi)
                st("sp", WTSD, wts[:], b_wts)
            P.flush()
        if stop <= 2:
            return nc

        with ExitStack() as s3:
            NS = CAP // 128
            wg_r = Rot([sbt(s3, "wg%d" % i, [128, 8, DFF], BF16) for i in range(2)])
            wu_r = Rot([sbt(s3, "wu%d" % i, [128, 8, DFF], BF16) for i in range(2)])
            wd_r = Rot([sbt(s3, "wd%d" % i, [128, 4, D], BF16) for i in range(2)])
            wds_r = Rot([sbt(s3, "wds%d" % i, [128, 4, D], F32) for i in range(2)])
            gt_r = Rot([sbt(s3, "gt%d" % i, [128, CAP // 128, D], BF16) for i in range(2)])
            GTr = Rot([sbt(s3, "GT%d" % i, [128, 8, CAP], BF16) for i in range(2)])
            actT, b_actT = sbt(s3, "actT", [128, 4, CAP], BF16)
            sg_r = Rot([sbt(s3, "sgB%d" % i, [128, CAP], F32) for i in range(2)])
            yb_r = Rot([sbt(s3, "yb%d" % i, [128, D], F32) for i in range(3)])
            sme_r = Rot([[sbt(s3, "sme%d_%d" % (i, j), [128, 1], I32) for j in range(NS)] for i in range(2)])

            def load_expert(e_):
                wg, b_wg = wg_r.next()
                wu, b_wu = wu_r.next()
                wd, b_wd = wd_r.next()
                gt, b_gt = gt_r.next()
                sme = sme_r.next()
                ld("sp", gt[:], G[e_ * CAP:(e_ + 1) * CAP, :].rearrange("(s p) d -> p s d", p=128), b_gt)
                ld("sp", wg[:], WG16[e_].rearrange("(k p) f -> p k f", p=128), b_wg)
                ld("sp", wu[:], WU16[e_].rearrange("(k p) f -> p k f", p=128), b_wu)
                wds, b_wds = wds_r.next()
                ld("sp", wds[:], w_down[e_].rearrange("(k p) f -> p k f", p=128), b_wds)
                casts.append((wd, b_wd, wds, b_wds))
                for j in range(NS):
                    r0 = e_ * CAP + j * 128
                    ld("sp", sme[j][0][:], SMAP[r0:r0 + 128, :], sme[j][1])
                return (wg, b_wg, wu, b_wu, wd, b_wd, gt, b_gt, sme)

            breg = {}
            casts = []
            b_Y2 = P.buf("Y2dram")

            gts = {}

            def expert_tr(e_, W, s_):
                gt, b_gt = W[6], W[7]
                if s_ == 0:
                    gts[e_] = GTr.next()
                GT, b_GT = gts[e_]
                pb, b_pb = PB.next()
                for k in range(8):
                    tr(pb[:, k, :], gt[:, s_, k * 128:(k + 1) * 128], [b_gt], [b_pb])
                cp("act" if s_ % 2 == 0 else "dve", GT[:, :, s_ * 128:(s_ + 1) * 128], pb[:, :, :],
                   [b_pb], [b_GT])

            def expert(e_, W, Wnext):
                wg, b_wg, wu, b_wu, wd, b_wd, gt, b_gt, sme = W
                GT, b_GT = gts.pop(e_)
                for f in range(4):
                    if Wnext is not None and 1 <= f <= NS:
                        expert_tr(e_ + 1, Wnext, f - 1)
                    pg, b_pg = PF.next()
                    pu, b_pu = PF.next()
                    for k in range(8):
                        mm(pg[:, 0:CAP], wg[:, k, f * 128:(f + 1) * 128], GT[:, k, :], k == 0, k == 7,
                           [b_wg, b_GT], [b_pg])
                    for k in range(8):
                        mm(pu[:, 0:CAP], wu[:, k, f * 128:(f + 1) * 128], GT[:, k, :], k == 0, k == 7,
                           [b_wu, b_GT], [b_pu])
                    sg, b_sg = sg_r.next()
                    act(sg[:], pg[:, 0:CAP], AF.Silu, [b_pg], [b_sg])
                    tt("dve", actT[:, f, :], sg[:], pu[:, 0:CAP], ALU.mult, [b_sg, b_pu], [b_actT])
                for s_ in range(NS):
                    yb, b_yb = yb_r.next()
                    for n in range(2):
                        po, b_po = PF.next()
                        for f in range(4):
                            mm(po[:, :], actT[:, f, s_ * 128:(s_ + 1) * 128], wd[:, f, n * 512:(n + 1) * 512],
                               f == 0, f == 3, [b_actT, b_wd], [b_po])
                        cp("act" if n == 0 else "dve", yb[:, n * 512:(n + 1) * 512], po[:, :], [b_po], [b_yb])
                    def scat(e, s_=s_, yb=yb):
                        return e.indirect_dma_start(
                            out=Y2, out_offset=bass.IndirectOffsetOnAxis(ap=sme[s_][0][:, :], axis=0),
                            in_=yb[:, :], in_offset=None)
                    P.dma("pool", scat, b_yb, False, reads=[sme[s_][1]])

            def do_casts():
                while casts:
                    wd, b_wd, wds, b_wds = casts.pop(0)
                    cp("act", wd[:, 0:2, :], wds[:, 0:2, :], [b_wds], [b_wd])
                    cp("dve", wd[:, 2:4, :], wds[:, 2:4, :], [b_wds], [b_wd])

            Wn = load_expert(0)
            do_casts()
            for s_ in range(NS):
                expert_tr(0, Wn, s_)
            for e_ in range(NE):
                W = Wn
                Wn = load_expert(e_ + 1) if e_ + 1 < NE else None
                expert(e_, W, Wn)
                do_casts()
            P.flush()
        if stop <= 3:
            return nc

        with ExitStack() as s4:
            gfrep, b_gf = sbt(s4, "gfrep", [128, D], F32)
            y12r = Rot([sbt(s4, "y12_%d" % i, [128, 2, D], F32) for i in range(4)])
            xmr = Rot([sbt(s4, "xmc%d" % i, [128, D], F32) for i in range(4)])
            acr = Rot([sbt(s4, "acc%d" % i, [128, D], F32) for i in range(3)])
            outr = Rot([sbt(s4, "o%d" % i, [128, D], F32) for i in range(3)])
            junk, b_junk = sbt(s4, "junk4", [128, D], BF16)
            ssr = Rot([sbt(s4, "ss4_%d" % i, [128, 4], F32) for i in range(2)])
            ld("sp", gfrep[:], gf_d, b_gf)
            loaded = {}

            def c_load(T):
                y12, b_y12 = y12r.next()
                xm, b_xm = xmr.next()
                ld("sp", y12[:], Y2[T * 256:(T + 1) * 256, :].rearrange("(p k) d -> p k d", k=2), b_y12)
                ld("sp", xm[:], XM[T * 128:(T + 1) * 128, :], b_xm)
                loaded[T] = (y12, b_y12, xm, b_xm)

            c_load(0)
            c_load(1)
            for T in range(NTILE):
                if T + 2 < NTILE:
                    c_load(T + 2)
                y12, b_y12, xm, b_xm = loaded.pop(T)
                acc, b_acc = acr.next()
                stt("dve", acc[:], y12[:, 0, :], wts[:, 2 * T:2 * T + 1], xm[:], ALU.mult, ALU.add,
                    [b_y12, b_wts, b_xm], [b_acc])
                stt("dve", acc[:], y12[:, 1, :], wts[:, 2 * T + 1:2 * T + 2], acc[:], ALU.mult, ALU.add,
                    [b_y12, b_wts, b_acc], [b_acc])
                ssq, b_ss = ssr.next()
                o, b_o = outr.next()
                act(junk[:], acc[:], AF.Square, [b_acc], [b_junk, b_ss], accum=ssq[:, 0:1])
                ts("dve", ssq[:, 1:2], ssq[:, 0:1], 1.0 / D, EPS, ALU.mult, ALU.add, [b_ss], [b_ss])
                act(ssq[:, 3:4], ssq[:, 1:2], AF.Ln, [b_ss], [b_ss])
                act(ssq[:, 2:3], ssq[:, 3:4], AF.Exp, [b_ss], [b_ss], scale=-0.5)
                stt("dve", o[:], acc[:], ssq[:, 2:3], gfrep[:], ALU.mult, ALU.mult, [b_acc, b_ss, b_gf], [b_o])
                st("sp", out[T * 128:(T + 1) * 128, :], o[:], b_o)
            P.flush()
    return nc


def _consts():
    q = np.arange(128)[:, None]
    s = np.arange(256)[None, :]
    dist = (q - s + 128).astype(np.float32)
    valid = (dist >= 0) & (dist < 128)
    slopes = 2.0 ** (-8.0 * (np.arange(NH) + 1) / NH)
    biasT = np.empty((128, 8, 512), np.float32)
    pk = np.arange(128)[:, None]
    qq = np.arange(128)[None, :]
    for c in range(8):
        for i in range(2):
            for kt in range(2):
                dkq = (qq - (kt * 128 + pk) + 128).astype(np.float32)
                ok = (dkq >= 0) & (dkq < 128)
                blk = (i * 2 + kt) * 128
                biasT[:, c, blk:blk + 128] = np.where(ok, slopes[2 * c + i] * dkq, 1.0e9)
    distm = biasT
    ident = np.eye(128, dtype=np.float32)
    tri = (np.arange(128)[:, None] < np.arange(128)[None, :]).astype(np.float32)
    ecap = np.broadcast_to((np.arange(NE) * CAP).astype(np.float32)[None, :], (128, NE)).copy()
    return distm, ident, tri, ecap


def make_in_maps(x, norm_mix, w_in, conv_w, conv_b, w_a_out, sinks, w_b_out, w_o, norm_ffn,
                 w_group, b_group, w_expert, b_expert, w_gate, w_up, w_down, norm_final):
    f = lambda a: np.ascontiguousarray(np.asarray(a, dtype=np.float32))
    x = f(x)
    bsz, seq, d = x.shape
    xf = x.reshape(bsz * seq, d)
    distm, ident, tri, ecap = _consts()
    cw = np.concatenate([f(conv_w).T, f(conv_b)[:, None]], axis=1)
    cw = np.ascontiguousarray(cw.reshape(8, 128, 4).transpose(1, 0, 2))
    rep = lambda v: np.ascontiguousarray(np.broadcast_to(f(v)[None, :], (128, f(v).shape[0])))
    wr = np.ascontiguousarray(np.concatenate([f(w_group), f(w_expert)], axis=1))
    br = rep(np.concatenate([f(b_group), f(b_expert)]))
    shared = {
        "w_in": f(w_in), "cw": cw, "w_a_out": f(w_a_out), "w_b_out": f(w_b_out), "w_o": f(w_o),
        "sinksrep": rep(sinks), "g1rep": rep(norm_mix), "g2rep": rep(norm_ffn), "gfrep": rep(norm_final),
        "wr": wr, "brrep": br, "w_gate": f(w_gate), "w_up": f(w_up), "w_down": f(w_down),
        "biasT": distm, "identc": ident, "tric": tri, "ecap": ecap,
    }
    tokidx = (np.arange(NTILE)[None, :] * 128 + np.arange(128)[:, None])
    ROWID = np.ascontiguousarray(
        (2 * tokidx[:, :, None] + np.arange(2)[None, None, :]).reshape(128, NTILE * 2).astype(np.int32))
    SMI = (2 * TOK + np.arange(NE * CAP, dtype=np.int32)).reshape(128, NE * CAP // 128)
    in_maps = []
    for c in range(NCORES):
        r0 = c * TOK
        xh = np.empty((TOK + 128, d), np.float32)
        first = (r0 % seq) == 0
        if first:
            xh[:128] = 0.0
        else:
            xh[:128] = xf[r0 - 128:r0]
        xh[128:] = xf[r0:r0 + TOK]
        m = dict(shared)
        m["rowid"] = ROWID
        m["smapinit"] = SMI
        m["xh"] = xh
        hm = np.zeros((128, 512), np.float32)
        if first:
            hm[:, 0:128] = 1.0e9
            hm[:, 256:384] = 1.0e9
        m["hmaskT"] = hm
        in_maps.append(m)
    return in_maps


_NC_CACHE = {}


def kernel(**inputs):
    x = np.asarray(inputs["x"])
    bsz, seq, d = x.shape
    assert (bsz, seq, d) == (4, 8192, 1024)
    in_maps = make_in_maps(**inputs)
    if "nc" not in _NC_CACHE:
        _NC_CACHE["nc"] = build_nc()
    nc = _NC_CACHE["nc"]
    res = run_bass_kernel_spmd(nc, in_maps, core_ids=list(range(NCORES)))
    outs = [np.asarray(r["out"], dtype=np.float32) for r in res.results]
    return np.concatenate(outs, axis=0).reshape(bsz, seq, d)
```
